# Optimizing a Trainium2 kernel written in Bass

```python
import jax, jax.numpy as jnp
from jax import lax
import numpy as np

D_MODEL = 2048
BATCH = 4
SEQ = 4096
DEPTH = 1

HG_HEADS = 8
HG_DK = 128
HG_DV = 128
HG_K = HG_HEADS * HG_DK
HG_V = HG_HEADS * HG_DV
HGRN_CHUNK = 64
GM_GROUPS = 8
GM_CH = 128
GM_WIDTH = GM_GROUPS * GM_CH
GM_CHUNK = 128
MIX_WIDTH = HG_V + GM_WIDTH
IN_SPLITS = (HG_K, 2 * HG_K, 2 * HG_K + HG_V, 2 * HG_K + 2 * HG_V, 2 * HG_K + 2 * HG_V + GM_WIDTH)
IN_WIDTH = 2 * HG_K + 2 * HG_V + 2 * GM_WIDTH
N_EXPERTS = 32
TOP_K = 4
D_FF = 2048
SWIGLU_LIMIT = 7.0
SWIGLU_ALPHA = 1.702
MOE_BLOCK = 256
PLE_DIM = 256
RMS_EPS = 1e-6
LN_EPS = 1e-5

kernel_name = "hybrid_hgrn2_gmlp_moe_block"


def rms_norm(x, g):
    xf = x.astype(jnp.float32)
    y = xf * lax.rsqrt(jnp.mean(xf * xf, axis=-1, keepdims=True) + RMS_EPS)
    return (y * g.astype(jnp.float32)).astype(x.dtype)


def layer_norm(x, g, b):
    xf = x.astype(jnp.float32)
    mu = jnp.mean(xf, axis=-1, keepdims=True)
    xc = xf - mu
    y = xc * lax.rsqrt(jnp.mean(xc * xc, axis=-1, keepdims=True) + LN_EPS)
    return (y * g.astype(jnp.float32) + b.astype(jnp.float32)).astype(x.dtype)


def hgrn2_recurrence(q, f_logit, v, lb):
    B, S = q.shape[0], q.shape[1]
    n = S // HGRN_CHUNK
    q = jax.nn.silu(q.astype(jnp.float32))
    f = lb + (1.0 - lb) * jax.nn.sigmoid(f_logit.astype(jnp.float32))
    log_f = jnp.log(f)
    k = 1.0 - f

    def to_chunks(t):
        return t.reshape(B, n, HGRN_CHUNK, HG_HEADS, t.shape[-1]).transpose(1, 0, 3, 2, 4)

    causal = jnp.tril(jnp.ones((HGRN_CHUNK, HGRN_CHUNK), dtype=bool))

    def step(state, inp):
        qc, kc, vc, gc = inp
        b = jnp.cumsum(gc, axis=2)
        o_inter = jnp.einsum('bhtd,bhde->bhte', qc * jnp.exp(b), state)
        diff = b[:, :, :, None, :] - b[:, :, None, :, :]
        decay = jnp.exp(jnp.where(causal[:, :, None], diff, -jnp.inf))
        scores = jnp.einsum('bhtd,bhsd,bhtsd->bhts', qc, kc, decay)
        o = o_inter + jnp.einsum('bhts,bhse->bhte', scores, vc)
        b_last = b[:, :, -1:, :]
        new_state = jnp.exp(b_last[:, :, 0, :])[..., None] * state + jnp.einsum(
            'bhsd,bhse->bhde', kc * jnp.exp(b_last - b), vc)
        return new_state, o

    s0 = jnp.zeros((B, HG_HEADS, HG_DK, HG_DV), jnp.float32)
    _, o = lax.scan(step, s0, (to_chunks(q), to_chunks(k), to_chunks(v.astype(jnp.float32)), to_chunks(log_f)))
    return o.transpose(1, 0, 3, 2, 4).reshape(B, S, HG_HEADS, HG_DV)


def chunked_spatial_gating(u, v, ln_g, ln_b, w_s, b_s):
    B, S = u.shape[0], u.shape[1]
    n = S // GM_CHUNK
    u = jax.nn.gelu(u, approximate=False)
    v = layer_norm(jax.nn.gelu(v, approximate=False), ln_g, ln_b)
    vb = v.reshape(B, n, GM_CHUNK, GM_GROUPS, GM_CH)
    w = w_s * jnp.tril(jnp.ones((GM_CHUNK, GM_CHUNK), w_s.dtype))
    mixed = jnp.einsum('gts,bnsgc->bntgc', w, vb) + b_s.T[:, :, None]
    return u * mixed.reshape(B, S, GM_WIDTH)


def moe_ffn(h, w_router, b_router, w_gu, b_gu, w_dn, b_dn):
    T, D = h.shape
    logits = (h @ w_router + b_router).astype(jnp.float32)
    top_val, top_idx = lax.top_k(logits, TOP_K)
    gates = jax.nn.softmax(top_val, axis=-1).astype(h.dtype)
    A = T * TOP_K
    e_flat = top_idx.reshape(-1).astype(jnp.int32)
    tok_flat = jnp.arange(A, dtype=jnp.int32) // TOP_K
    g_flat = gates.reshape(-1)
    order = jnp.argsort(e_flat)
    e_sorted, tok_sorted, g_sorted = e_flat[order], tok_flat[order], g_flat[order]
    counts = jnp.zeros((N_EXPERTS,), jnp.int32).at[e_flat].add(1)
    starts = jnp.cumsum(counts) - counts
    padded = (counts + MOE_BLOCK - 1) // MOE_BLOCK * MOE_BLOCK
    pad_ends = jnp.cumsum(padded)
    pad_starts = pad_ends - padded
    dest = pad_starts[e_sorted] + (jnp.arange(A, dtype=jnp.int32) - starts[e_sorted])
    n_blocks = -(-A // MOE_BLOCK) + N_EXPERTS
    n_rows = n_blocks * MOE_BLOCK
    row_tok = jnp.zeros((n_rows,), jnp.int32).at[dest].set(tok_sorted)
    row_gate = jnp.zeros((n_rows,), h.dtype).at[dest].set(g_sorted)
    block_start = jnp.arange(n_blocks, dtype=jnp.int32) * MOE_BLOCK
    block_expert = jnp.minimum(jnp.searchsorted(pad_ends, block_start, side='right'), N_EXPERTS - 1)

    def expert_block(args):
        toks, gts, e = args
        xb = h[toks]
        gu = xb @ w_gu[e] + b_gu[e]
        gate = jnp.minimum(gu[:, 0::2], SWIGLU_LIMIT)
        up = jnp.clip(gu[:, 1::2], -SWIGLU_LIMIT, SWIGLU_LIMIT)
        act = (up + 1.0) * gate * jax.nn.sigmoid(SWIGLU_ALPHA * gate)
        y = act @ w_dn[e] + b_dn[e]
        return y * gts[:, None]

    ys = lax.map(expert_block, (row_tok.reshape(n_blocks, MOE_BLOCK),
                                row_gate.reshape(n_blocks, MOE_BLOCK), block_expert))
    return jax.ops.segment_sum(ys.reshape(n_rows, D), row_tok, num_segments=T)


def setup_inputs(seed: int = 0) -> dict:
    key = jax.random.key(seed)
    ks = jax.random.split(key, 24)
    f32 = jnp.float32
    nrm = lambda k, s: jax.random.normal(k, s, f32)
    L = DEPTH
    return {
        "x": nrm(ks[0], (BATCH, SEQ, D_MODEL)),
        "p": nrm(ks[1], (DEPTH, BATCH, SEQ, PLE_DIM)),
        "norm_mix": 1.0 + 0.02 * nrm(ks[2], (L, D_MODEL)),
        "w_in": nrm(ks[3], (L, D_MODEL, IN_WIDTH)) * D_MODEL ** -0.5,
        "lb_logits": 0.5 * nrm(ks[4], (DEPTH + 1, HG_K)),
        "hgrn_out_norm": 1.0 + 0.02 * nrm(ks[5], (L, HG_V)),
        "gmlp_ln_g": 1.0 + 0.02 * nrm(ks[6], (L, GM_WIDTH)),
        "gmlp_ln_b": 0.02 * nrm(ks[7], (L, GM_WIDTH)),
        "w_spatial": nrm(ks[8], (L, GM_GROUPS, GM_CHUNK, GM_CHUNK)) * GM_CHUNK ** -0.5,
        "b_spatial": 1.0 + 0.02 * nrm(ks[9], (L, GM_GROUPS, GM_CHUNK)),
        "w_out": nrm(ks[10], (L, MIX_WIDTH, D_MODEL)) * MIX_WIDTH ** -0.5,
        "norm_ffn": 1.0 + 0.02 * nrm(ks[11], (L, D_MODEL)),
        "w_router": nrm(ks[12], (L, D_MODEL, N_EXPERTS)) * D_MODEL ** -0.5,
        "b_router": 0.01 * nrm(ks[13], (L, N_EXPERTS)),
        "w_gate_up": nrm(ks[14], (L, N_EXPERTS, D_MODEL, 2 * D_FF)) * D_MODEL ** -0.5,
        "b_gate_up": 0.02 * nrm(ks[15], (L, N_EXPERTS, 2 * D_FF)),
        "w_down": nrm(ks[16], (L, N_EXPERTS, D_FF, D_MODEL)) * D_FF ** -0.5,
        "b_down": 0.02 * nrm(ks[17], (L, N_EXPERTS, D_MODEL)),
        "norm_ple": 1.0 + 0.02 * nrm(ks[18], (L, D_MODEL)),
        "w_ple_gate": nrm(ks[19], (L, D_MODEL, D_MODEL)) * D_MODEL ** -0.5,
        "w_ple_proj": nrm(ks[20], (L, PLE_DIM, D_MODEL)) * PLE_DIM ** -0.5,
        "norm_final": 1.0 + 0.02 * nrm(ks[21], (D_MODEL,)),
    }


def reference(x, p, norm_mix, w_in, lb_logits, hgrn_out_norm, gmlp_ln_g, gmlp_ln_b, w_spatial,
              b_spatial, w_out, norm_ffn, w_router, b_router, w_gate_up, b_gate_up, w_down, b_down,
              norm_ple, w_ple_gate, w_ple_proj, norm_final):
    B, S, D = x.shape
    lower_bounds = jnp.cumsum(jax.nn.softmax(lb_logits.astype(jnp.float32), axis=0), axis=0)
    for i in range(DEPTH):
        h = rms_norm(x, norm_mix[i])
        z = h @ w_in[i]
        q, f_logit, v_in, o_gate, u, v = jnp.split(z, IN_SPLITS, axis=-1)
        lb = lower_bounds[i].reshape(HG_HEADS, HG_DK)
        o = hgrn2_recurrence(q.reshape(B, S, HG_HEADS, HG_DK), f_logit.reshape(B, S, HG_HEADS, HG_DK),
                             v_in.reshape(B, S, HG_HEADS, HG_DV), lb)
        o = rms_norm(o, hgrn_out_norm[i].reshape(HG_HEADS, HG_DV)).reshape(B, S, HG_V).astype(x.dtype)
        o = o * jax.nn.sigmoid(o_gate)
        sg = chunked_spatial_gating(u, v, gmlp_ln_g[i], gmlp_ln_b[i], w_spatial[i], b_spatial[i])
        x = x + jnp.concatenate([o, sg], axis=-1) @ w_out[i]
        h = rms_norm(x, norm_ffn[i])
        x = x + moe_ffn(h.reshape(B * S, D), w_router[i], b_router[i], w_gate_up[i], b_gate_up[i],
                        w_down[i], b_down[i]).reshape(B, S, D)
        gate = jax.nn.sigmoid(rms_norm(x, norm_ple[i]) @ w_ple_gate[i])
        x = x + gate * (p[i] @ w_ple_proj[i])
    return rms_norm(x, norm_final)
```

```python
import numpy as np
from contextlib import ExitStack
import concourse.bass as bass
import concourse.mybir as mybir
from concourse.bass_utils import run_bass_kernel_spmd

F32 = mybir.dt.float32
BF16 = mybir.dt.bfloat16
AF = mybir.ActivationFunctionType
ALU = mybir.AluOpType
AX = mybir.AxisListType

D = 2048
NT_OWN = 16
NT_PRE = 16
GT = 4
TG = 8
NE = 32
RMS_EPS = 1e-6
LN_EPS = 1e-5
DEBUG = False
STOP = 0
SUB = 0


class Eng:
    def __init__(self, e, sem, is_pe=False):
        self.e = e
        self.sem = sem
        self.cnt = 0
        self.seen = {}
        self.is_pe = is_pe

    def wait(self, tok):
        if tok is None:
            return
        sem, val = tok
        if sem is self.sem and self.is_pe:
            return
        key = id(sem)
        if self.seen.get(key, 0) >= val:
            return
        self.e.wait_ge(sem, val)
        self.seen[key] = val

    def done(self, ins):
        ins.then_inc(self.sem, 1)
        self.cnt += 1
        return (self.sem, self.cnt)

    def future(self):
        return (self.sem, self.cnt + 1)


class Buf:
    def __init__(self, name="", excl=False):
        self.name = name
        self.excl = excl
        self.w = None
        self.r = {}
        self.dsem = None
        self.dcnt = 0


def _deps(E, reads, writes):
    for b in reads:
        E.wait(b.w)
        if b.excl:
            for t in list(b.r.values()):
                if t[0] is not E.sem:
                    E.wait(t)
    for b in writes:
        E.wait(b.w)
        for t in list(b.r.values()):
            E.wait(t)


def _upd(tok, reads, writes):
    for b in reads:
        k = id(tok[0])
        if k not in b.r or b.r[k][1] < tok[1]:
            b.r[k] = tok
    for b in writes:
        b.w = tok
        b.r = {}


def op(E, fn, reads=(), writes=(), mile=True):
    _deps(E, reads, writes)
    ins = fn()
    tok = E.done(ins) if mile else E.future()
    _upd(tok, reads, writes)
    return tok


def build_nc():
    nc = bass.Bass("TRN2", target_bir_lowering=False)

    def din(name, shape):
        return nc.dram_tensor(name, shape, F32, kind="ExternalInput").ap()

    xs = din("xs", [(NT_PRE + NT_OWN) * 128, D])
    pin = din("p", [NT_OWN * 128, 256])
    w_in = din("w_in", [D, 6144])
    w_out = din("w_out", [D, D])
    w_gu = din("w_gu", [NE, D, 4096])
    w_dn = din("w_dn", [NE, D, D])
    w_pg = din("w_pg", [D, D])
    w_pp = din("w_pp", [256, D])
    w_r = din("w_r", [D, NE])
    norm_mix = din("norm_mix", [D])
    norm_ffn = din("norm_ffn", [D])
    norm_ple = din("norm_ple", [D])
    norm_final = din("norm_final", [D])
    lb_logits = din("lb_logits", [2, 1024])
    hon_t = din("hon_t", [128, 8])
    ln_g = din("ln_g", [1024])
    ln_b = din("ln_b", [1024])
    ws_t = din("ws_t", [128, 8, 128])
    bs_t = din("bs_t", [128, 8])
    b_r = din("b_r", [NE])
    bgu_t = din("bgu_t", [128, NE * 2 * 16])
    b_dn = din("b_dn", [NE, D])
    out = nc.dram_tensor("out", [NT_OWN * 128, D], F32, kind="ExternalOutput").ap()
    x1s = nc.dram_tensor("x1s", [NT_OWN * 128, D], F32, kind=("ExternalOutput" if DEBUG else "Internal")).ap()

    es = ExitStack()
    with es:
        def sem(name):
            return es.enter_context(nc.semaphore(name))

        PE = Eng(nc.tensor, sem("s_pe"), is_pe=True)
        ACT = Eng(nc.scalar, sem("s_act"))
        DVE = Eng(nc.vector, sem("s_dve"))
        POOL = Eng(nc.gpsimd, sem("s_pool"))
        SP = Eng(nc.sync, sem("s_sp"))

        def dma(out_ap, in_ap, reads, writes, sb):
            if sb.dsem is None:
                sb.dsem = sem("d_" + sb.name)
            _deps(SP, reads, writes)
            SP.e.dma_start(out=out_ap, in_=in_ap).then_inc(sb.dsem, 16)
            sb.dcnt += 16
            tok = (sb.dsem, sb.dcnt)
            _upd(tok, reads, writes)
            return tok

        x1b = [Buf("x1s%d" % i) for i in range(NT_OWN)]
        out_toks = []

        with ExitStack() as pa:
            def sb(name, shape, dt=F32):
                return pa.enter_context(nc.sbuf_tensor(name, shape, dt)), Buf(name)

            def psb(name, shape, dt=F32):
                return pa.enter_context(nc.psum_tensor(name, shape, dt))

            X = [sb("X%d" % i, [128, D]) for i in range(GT)]
            hT, hTb = sb("hT", [128, 16, GT * 128], BF16)
            hb, hbb = sb("hb", [128, D], BF16)
            stg = [sb("stg%d" % i, [128, 16, 128]) for i in range(3)]
            wb = [sb("wb%d" % i, [128, 16, 512], BF16) for i in range(2)]
            mix = [sb("mix%d" % i, [128, D], BF16) for i in range(GT)]
            GU = [sb("GU%d" % i, [128, 1024]) for i in range(GT)]
            GV = [sb("GV%d" % i, [128, 1024]) for i in range(GT)]
            vn, vnb = sb("vn", [128, 1024], BF16)
            nmB, nmBb = sb("nmB", [128, D])
            lbB, lbBb = sb("lbB", [128, 1024])
            omlB, omlBb = sb("omlB", [128, 1024])
            lngB, lngBb = sb("lngB", [128, 1024])
            lnbB, lnbBb = sb("lnbB", [128, 1024])
            wsT, wsTb = sb("wsT", [128, 8, 128], BF16)
            bsT, bsTb = sb("bsT", [128, 8])
            hon, honb = sb("hon", [128, 8])
            idb, idbb = sb("idb", [128, 128], BF16)
            triF, triFb = sb("triF", [128, 128])
            ind, indb = sb("ind", [128, 2])
            S = [sb("S%d" % i, [128, 128]) for i in range(8)]
            Sb = [sb("Sb%d" % i, [128, 128], BF16) for i in range(8)]
            def mkset(i):
                d = {}
                for nm in ["sf", "sq", "sgt", "ff", "lf", "kk", "enb", "eb", "qs", "tmpS"]:
                    d[nm] = sb("%s_%d" % (nm, i), [128, 128])
                d["dec"] = sb("dec_%d" % i, [128, 2])
                for nm in ["vbt", "ke", "qe", "keT", "qeT", "scm"]:
                    d[nm] = sb("%s_%d" % (nm, i), [128, 128], BF16)
                d["u3"] = sb("u3_%d" % i, [128, 1])
                d["u4"] = sb("u4_%d" % i, [128, 1])
                return d
            TS = [mkset(0), mkset(1)]
            uctr = [0]
            st1, st1b = sb("st1", [128, 1]); st2, st2b = sb("st2", [128, 1]); st3, st3b = sb("st3", [128, 1])
            st4, st4b = sb("st4", [128, 1]); st5, st5b = sb("st5", [128, 1])

            Z = [(psb("Z%d" % i, [128, 512]), Buf("Z%d" % i, True)) for i in range(2)]
            UA = [psb("UA%d" % i, [128, 512]) for i in range(2)]
            UAb = [Buf("UA%d" % i, True) for i in range(2)]
            UB = [psb("UB%d" % i, [128, 512]) for i in range(2)]
            UBb = [Buf("UB%d" % i, True) for i in range(2)]
            SPa = Buf("SPa", True)
            SPbk = Buf("SPbk", True)
            SPp = psb("SPp", [128, 1024])

            cst = Buf("cst")

            dma(nmB[:], norm_mix.partition_broadcast(128), [], [nmBb], nmBb)
            dma(lbB[:], lb_logits[0, :].partition_broadcast(128), [], [lbBb], lbBb)
            dma(omlB[:], lb_logits[1, :].partition_broadcast(128), [], [omlBb], omlBb)
            dma(lngB[:], ln_g.partition_broadcast(128), [], [lngBb], lngBb)
            dma(lnbB[:], ln_b.partition_broadcast(128), [], [lnbBb], lnbBb)
            dma(bsT[:], bs_t[:, :], [], [bsTb], bsTb)
            dma(hon[:], hon_t[:, :], [], [honb], honb)
            gu0v = GU[0][0][:].rearrange("p (g t) -> p g t", g=8)
            dma(gu0v, ws_t[:, :, :], [], [GU[0][1]], GU[0][1])
            op(POOL, lambda: nc.gpsimd.affine_select(out=gu0v, in_=gu0v, pattern=[[0, 8], [1, 128]],
                                                     compare_op=ALU.is_ge, fill=0.0, base=0, channel_multiplier=-1),
               [GU[0][1]], [GU[0][1]])
            op(POOL, lambda: nc.gpsimd.tensor_copy(out=wsT[:], in_=gu0v), [GU[0][1]], [wsTb])
            op(POOL, lambda: nc.gpsimd.memset(idb[:], 1.0), [], [idbb])
            op(POOL, lambda: nc.gpsimd.affine_select(out=idb[:], in_=idb[:], pattern=[[-1, 128]], compare_op=ALU.is_equal,
                                                     fill=0.0, base=0, channel_multiplier=1), [idbb], [idbb])
            op(POOL, lambda: nc.gpsimd.memset(triF[:], 1.0), [], [triFb])
            op(POOL, lambda: nc.gpsimd.affine_select(out=triF[:], in_=triF[:], pattern=[[1, 128]], compare_op=ALU.is_ge,
                                                     fill=0.0, base=0, channel_multiplier=-1), [triFb], [triFb])
            op(POOL, lambda: nc.gpsimd.memset(triF[0:64, 64:128], 0.0), [triFb], [triFb])
            op(POOL, lambda: nc.gpsimd.memset(ind[:], 0.0), [], [indb])
            op(POOL, lambda: nc.gpsimd.memset(ind[0:64, 0:1], 1.0), [indb], [indb])
            op(POOL, lambda: nc.gpsimd.memset(ind[64:128, 1:2], 1.0), [indb], [indb])
            for h in range(8):
                op(POOL, lambda h=h: nc.gpsimd.memset(S[h][0][:], 0.0), [], [S[h][1]])
                op(POOL, lambda h=h: nc.gpsimd.memset(Sb[h][0][:], 0.0), [], [Sb[h][1]])
            op(DVE, lambda: nc.vector.tensor_tensor(out=lbB[:], in0=lbB[:], in1=omlB[:], op=ALU.subtract), [lbBb, omlBb], [lbBb])
            op(ACT, lambda: nc.scalar.activation(out=lbB[:], in_=lbB[:], func=AF.Sigmoid), [lbBb], [lbBb])
            op(DVE, lambda: nc.vector.tensor_scalar(out=omlB[:], in0=lbB[:], scalar1=-1.0, scalar2=1.0, op0=ALU.mult, op1=ALU.add),
               [lbBb], [omlBb])

            if STOP == 1:
                return nc

            def rstd_from_sum(src, srcb, dst, dstb, n, eps):
                op(DVE, lambda: nc.vector.tensor_scalar(out=dst[:], in0=src[:], scalar1=1.0 / n, scalar2=eps,
                                                        op0=ALU.mult, op1=ALU.add), [srcb], [dstb])
                op(ACT, lambda: nc.scalar.activation(out=dst[:], in_=dst[:], func=AF.Ln), [dstb], [dstb])
                op(ACT, lambda: nc.scalar.activation(out=dst[:], in_=dst[:], func=AF.Exp, scale=-0.5), [dstb], [dstb])

            def transpose16(src, srcb, dstT, dstTb, tcol):
                for q4 in range(4):
                    TRt, trb = UB[q4 % 2], UBb[q4 % 2]
                    for j in range(4):
                        c = q4 * 4 + j
                        op(PE, lambda c=c, j=j: nc.tensor.matmul(TRt[:, j * 128:(j + 1) * 128],
                                                                 src[:, c * 128:(c + 1) * 128], idb[:], start=True, stop=True),
                           [srcb, idbb], [trb], mile=(j == 3))
                    srcv = TRt[:, 0:512].rearrange("p (c t) -> p c t", c=4)
                    dstv = dstT[:, q4 * 4:(q4 + 1) * 4, tcol:tcol + 128]
                    if q4 % 2 == 0:
                        op(ACT, lambda: nc.scalar.copy(out=dstv, in_=srcv), [trb], [dstTb])
                    else:
                        op(DVE, lambda: nc.vector.tensor_copy(out=dstv, in_=srcv), [trb], [dstTb])

            wstate = {"stg": 0, "wb": 0}

            def load_block(wmat, cols, fold_hon=False):
                slot = wstate["wb"] % 2
                wstate["wb"] += 1
                wbt, wbb = wb[slot]
                for j, c0 in enumerate(cols):
                    s = wstate["stg"] % 3
                    wstate["stg"] += 1
                    stt, stb = stg[s]
                    dma(stt[:], wmat[:, c0:c0 + 128].rearrange("(c p) n -> p c n", p=128), [], [stb], stb)
                    if fold_hon:
                        for c in range(8):
                            if c % 2 == 0:
                                op(POOL, lambda c=c: nc.gpsimd.tensor_scalar(out=wbt[:, c, j * 128:(j + 1) * 128], in0=stt[:, c, :],
                                                                               scalar1=hon[:, c:c + 1], scalar2=None, op0=ALU.mult),
                                   [stb, honb], [wbb])
                            else:
                                op(DVE, lambda c=c: nc.vector.tensor_scalar(out=wbt[:, c, j * 128:(j + 1) * 128], in0=stt[:, c, :],
                                                                              scalar1=hon[:, c:c + 1], scalar2=None, op0=ALU.mult),
                                   [stb, honb], [wbb])
                        op(DVE if j % 2 == 0 else POOL,
                           (lambda: nc.vector.tensor_copy(out=wbt[:, 8:16, j * 128:(j + 1) * 128], in_=stt[:, 8:16, :])) if j % 2 == 0 else
                           (lambda: nc.gpsimd.tensor_copy(out=wbt[:, 8:16, j * 128:(j + 1) * 128], in_=stt[:, 8:16, :])),
                           [stb], [wbb])
                    elif j % 2 == 0:
                        op(POOL, lambda: nc.gpsimd.tensor_copy(out=wbt[:, :, j * 128:(j + 1) * 128], in_=stt[:, :, :]),
                           [stb], [wbb])
                    else:
                        op(DVE, lambda: nc.vector.tensor_copy(out=wbt[:, :, j * 128:(j + 1) * 128], in_=stt[:, :, :]),
                           [stb], [wbb])
                return wbt, wbb

            def inproj(t, wbt, wbb, ncols, zi):
                zt, zb = Z[zi]
                for c in range(16):
                    op(PE, lambda c=c: nc.tensor.matmul(zt[:, 0:ncols], hT[:, c, t * 128:(t + 1) * 128], wbt[:, c, 0:ncols],
                                                        start=(c == 0), stop=(c == 15)),
                       [hTb, wbb], [zb], mile=(c == 15))
                return zt, zb

            def hgrn_unit(t, hd, own, zb, zq, zf, zv, zg):
                hs = slice(hd * 128, (hd + 1) * 128)
                tset = TS[uctr[0] % 2]
                uctr[0] += 1
                (sf, sfb), (sq, sqb), (sgt, sgtb), (ff, ffb), (lf, lfb), (kk, kkb) = (tset[k_] for k_ in ["sf", "sq", "sgt", "ff", "lf", "kk"])
                (enb, enbb), (eb, ebb), (qs, qsb), (tmpS, tmpSb), (dec, decb) = (tset[k_] for k_ in ["enb", "eb", "qs", "tmpS", "dec"])
                (vbt, vbtb), (ke, keb), (qe, qeb), (keT, keTb), (qeT, qeTb), (scm, scmb) = (tset[k_] for k_ in ["vbt", "ke", "qe", "keT", "qeT", "scm"])
                (u3, u3b), (u4, u4b) = tset["u3"], tset["u4"]
                u_ = (uctr[0] - 1) % 2
                UAt, UAk, UBt, UBk = UA[u_], UAb[u_], UB[u_], UBb[u_]
                op(ACT, lambda: nc.scalar.activation(out=sf[:], in_=zf, func=AF.Sigmoid), [zb], [sfb])
                if own:
                    op(ACT, lambda: nc.scalar.activation(out=sq[:], in_=zq, func=AF.Sigmoid), [zb], [sqb])
                    op(ACT, lambda: nc.scalar.activation(out=sgt[:], in_=zg, func=AF.Sigmoid), [zb], [sgtb])
                op(ACT, lambda: nc.scalar.copy(out=vbt[:], in_=zv), [zb], [vbtb])
                op(DVE, lambda: nc.vector.tensor_tensor(out=ff[:], in0=sf[:], in1=omlB[:, hs], op=ALU.mult), [sfb, omlBb], [ffb])
                op(DVE, lambda: nc.vector.tensor_tensor(out=ff[:], in0=ff[:], in1=lbB[:, hs], op=ALU.add), [ffb, lbBb], [ffb])
                op(ACT, lambda: nc.scalar.activation(out=lf[:], in_=ff[:], func=AF.Ln), [ffb], [lfb])
                op(POOL, lambda: nc.gpsimd.tensor_scalar(out=kk[:], in0=ff[:], scalar1=-1.0, scalar2=1.0, op0=ALU.mult, op1=ALU.add),
                   [ffb], [kkb])
                if own:
                    op(DVE, lambda: nc.vector.tensor_tensor(out=qs[:], in0=zq, in1=sq[:], op=ALU.mult), [zb, sqb], [qsb])
                op(PE, lambda: nc.tensor.matmul(UAt[:, 0:128], triF[:], lf[:], start=True, stop=True), [triFb, lfb], [UAk])
                op(PE, lambda: nc.tensor.matmul(UAt[:, 384:386], lf[:], ind[:], start=True, stop=True), [lfb, indb], [UAk])
                op(ACT, lambda: nc.scalar.activation(out=enb[:], in_=UAt[:, 0:128], func=AF.Exp, scale=-1.0), [UAk], [enbb])
                if own:
                    op(ACT, lambda: nc.scalar.activation(out=eb[:], in_=UAt[:, 0:128], func=AF.Exp), [UAk], [ebb])
                op(ACT, lambda: nc.scalar.activation(out=dec[:], in_=UAt[:, 384:386], func=AF.Exp), [UAk], [decb])
                op(DVE, lambda: nc.vector.tensor_tensor(out=ke[:], in0=kk[:], in1=enb[:], op=ALU.mult), [kkb, enbb], [keb])
                if own:
                    op(DVE, lambda: nc.vector.tensor_tensor(out=qe[:], in0=qs[:], in1=eb[:], op=ALU.mult), [qsb, ebb], [qeb])
                    op(PE, lambda: nc.tensor.matmul(UBt[:, 0:128], ke[:], idb[:], start=True, stop=True), [keb, idbb], [UBk])
                    op(PE, lambda: nc.tensor.matmul(UBt[:, 128:256], qe[:], idb[:], start=True, stop=True), [qeb, idbb], [UBk])
                    op(ACT, lambda: nc.scalar.copy(out=keT[:], in_=UBt[:, 0:128]), [UBk], [keTb])
                    op(DVE, lambda: nc.vector.tensor_copy(out=qeT[:], in_=UBt[:, 128:256]), [UBk], [qeTb])
                    op(PE, lambda: nc.tensor.matmul(UAt[:, 128:256], keT[:], qeT[:], start=True, stop=True), [keTb, qeTb], [UAk])
                    op(DVE, lambda: nc.vector.tensor_tensor(out=scm[:], in0=UAt[:, 128:256], in1=triF[:], op=ALU.mult),
                       [UAk, triFb], [scmb])
                St, Stb = S[hd]
                Sbt, Sbb = Sb[hd]
                for ch in range(2):
                    rs = slice(ch * 64, (ch + 1) * 64)
                    if own:
                        op(PE, lambda: nc.tensor.matmul(UAt[rs, 256:384], scm[rs, rs], vbt[rs, :], start=True, stop=False),
                           [scmb, vbtb], [UAk], mile=False)
                        op(PE, lambda: nc.tensor.matmul(UAt[rs, 256:384], qeT[:, rs], Sbt[:], start=False, stop=True),
                           [qeTb, Sbb], [UAk])
                    su = UBt[:, 256 + ch * 128:256 + (ch + 1) * 128]
                    op(PE, lambda: nc.tensor.matmul(su, ke[rs, :], vbt[rs, :], start=True, stop=True), [keb, vbtb], [UBk])
                    op(DVE, lambda: nc.vector.tensor_tensor(out=tmpS[:], in0=su, in1=St[:], op=ALU.add), [UBk, Stb], [tmpSb])
                    op(DVE, lambda: nc.vector.tensor_scalar(out=St[:], in0=tmpS[:], scalar1=dec[:, ch:ch + 1], scalar2=None, op0=ALU.mult),
                       [tmpSb, decb], [Stb])
                    op(POOL, lambda: nc.gpsimd.tensor_copy(out=Sbt[:], in_=St[:]), [Stb], [Sbb])
                if own:
                    op(ACT, lambda: nc.scalar.activation(out=qs[:], in_=UAt[:, 256:384], func=AF.Square, accum_out=u3[:]),
                       [UAk], [qsb, u3b])
                    rstd_from_sum(u3, u3b, u4, u4b, 128, RMS_EPS)
                    op(DVE, lambda: nc.vector.scalar_tensor_tensor(out=mix[t][0][:, hs], in0=UAt[:, 256:384], scalar=u4[:, 0:1],
                                                                   in1=sgt[:], op0=ALU.mult, op1=ALU.mult),
                       [UAk, u4b, sgtb], [mix[t][1]])
            zctr = [0]
            for g in range((NT_PRE + NT_OWN) // GT):
                own = g >= NT_PRE // GT
                for t in range(GT):
                    Xt, Xb = X[t]
                    T = g * GT + t
                    dma(Xt[:], xs[T * 128:(T + 1) * 128, :], [], [Xb], Xb)
                    op(ACT, lambda: nc.scalar.activation(out=hb[:], in_=Xt[:], func=AF.Square, accum_out=st1[:]),
                       [Xb], [hbb, st1b])
                    if SUB == 1:
                        return nc
                    rstd_from_sum(st1, st1b, st2, st2b, D, RMS_EPS)
                    if SUB == 2:
                        return nc
                    op(DVE, lambda: nc.vector.scalar_tensor_tensor(out=hb[:], in0=Xt[:], scalar=st2[:, 0:1], in1=nmB[:],
                                                                   op0=ALU.mult, op1=ALU.mult), [Xb, st2b, nmBb], [hbb])
                    if SUB == 3:
                        return nc
                    transpose16(hb, hbb, hT, hTb, t * 128)
                if STOP == 2:
                    return nc
                blocks = []
                for hd in range(8):
                    if own:
                        blocks.append(("head", hd, [hd * 128, 1024 + hd * 128, 2048 + hd * 128, 3072 + hd * 128]))
                    elif hd % 2 == 0:
                        blocks.append(("head2", hd // 2, [1024 + hd * 128, 2048 + hd * 128, 1024 + (hd + 1) * 128, 2048 + (hd + 1) * 128]))
                if own:
                    for ub in range(2):
                        blocks.append(("u", ub, [4096 + ub * 512 + j * 128 for j in range(4)]))
                    for vbk in range(2):
                        blocks.append(("v", vbk, [5120 + vbk * 512 + j * 128 for j in range(4)]))
                loaded = {}
                loaded[0] = load_block(w_in, blocks[0][2])
                for bi, (kind, idx, cols) in enumerate(blocks):
                    if bi + 1 < len(blocks):
                        loaded[bi + 1] = load_block(w_in, blocks[bi + 1][2])
                    wbt, wbb = loaded.pop(bi)
                    ncols = len(cols) * 128
                    for t in range(GT):
                        zi = zctr[0] % 2
                        zctr[0] += 1
                        zt, zb = inproj(t, wbt, wbb, ncols, zi)
                        if SUB == 11:
                            return nc
                        if kind == "u":
                            op(ACT, lambda: nc.scalar.activation(out=GU[t][0][:, idx * 512:(idx + 1) * 512], in_=zt[:, 0:512], func=AF.Gelu),
                               [zb], [GU[t][1]])
                            continue
                        if kind == "v":
                            op(ACT, lambda: nc.scalar.activation(out=GV[t][0][:, idx * 512:(idx + 1) * 512], in_=zt[:, 0:512], func=AF.Gelu),
                               [zb], [GV[t][1]])
                            continue
                        if kind == "head":
                            hgrn_unit(t, idx, True, zb, zt[:, 0:128], zt[:, 128:256], zt[:, 256:384], zt[:, 384:512])
                        else:
                            for k2 in range(2):
                                hgrn_unit(t, idx * 2 + k2, False, zb, None, zt[:, k2 * 256:k2 * 256 + 128],
                                          zt[:, k2 * 256 + 128:k2 * 256 + 256], None)
                    if STOP == 3:
                        return nc
                if STOP == 4:
                    return nc
                if not own:
                    continue
                if STOP == 5:
                    return nc
                for t in range(GT):
                    GVt, GVb = GV[t]
                    op(DVE, lambda: nc.vector.reduce_sum(out=st1[:], in_=GVt[:], axis=AX.X), [GVb], [st1b])
                    op(ACT, lambda: nc.scalar.activation(out=hb[:, 0:1024], in_=GVt[:], func=AF.Square, accum_out=st3[:]),
                       [GVb], [hbb, st3b])
                    op(DVE, lambda: nc.vector.tensor_scalar(out=st1[:], in0=st1[:], scalar1=1.0 / 1024, scalar2=None, op0=ALU.mult),
                       [st1b], [st1b])
                    op(DVE, lambda: nc.vector.tensor_tensor(out=st5[:], in0=st1[:], in1=st1[:], op=ALU.mult), [st1b], [st5b])
                    op(DVE, lambda: nc.vector.scalar_tensor_tensor(out=st3[:], in0=st3[:], scalar=1.0 / 1024, in1=st5[:],
                                                                   op0=ALU.mult, op1=ALU.subtract), [st3b, st5b], [st3b])
                    rstd_from_sum(st3, st3b, st4, st4b, 1.0, LN_EPS)
                    op(DVE, lambda: nc.vector.tensor_scalar(out=GVt[:], in0=GVt[:], scalar1=st1[:, 0:1], scalar2=st4[:, 0:1],
                                                            op0=ALU.subtract, op1=ALU.mult), [GVb, st1b, st4b], [GVb])
                    op(DVE, lambda: nc.vector.tensor_tensor(out=GVt[:], in0=GVt[:], in1=lngB[:], op=ALU.mult), [GVb, lngBb], [GVb])
                    op(DVE, lambda: nc.vector.tensor_tensor(out=vn[:], in0=GVt[:], in1=lnbB[:], op=ALU.add), [GVb, lnbBb], [vnb])
                    for g8 in range(8):
                        gs = slice(g8 * 128, (g8 + 1) * 128)
                        op(PE, lambda: nc.tensor.matmul(SPp[:, gs], wsT[:, g8, :], vn[:, gs], start=True, stop=True),
                           [wsTb, vnb], [SPa if g8 < 4 else SPbk], mile=(g8 == 3 or g8 == 7))
                    for g8 in range(8):
                        gs = slice(g8 * 128, (g8 + 1) * 128)
                        ms = slice(1024 + g8 * 128, 1024 + (g8 + 1) * 128)
                        op(DVE, lambda: nc.vector.scalar_tensor_tensor(out=mix[t][0][:, ms], in0=SPp[:, gs], scalar=bsT[:, g8:g8 + 1],
                                                                       in1=GU[t][0][:, gs], op0=ALU.add, op1=ALU.mult),
                           [SPa if g8 < 4 else SPbk, bsTb, GU[t][1]], [mix[t][1]])
                if STOP == 6:
                    return nc
                for t in range(GT):
                    transpose16(mix[t][0], mix[t][1], hT, hTb, t * 128)
                oblocks = [[db * 512 + j * 128 for j in range(4)] for db in range(4)]
                loaded = {0: load_block(w_out, oblocks[0], fold_hon=True)}
                for db in range(4):
                    if db + 1 < 4:
                        loaded[db + 1] = load_block(w_out, oblocks[db + 1], fold_hon=True)
                    wbt, wbb = loaded.pop(db)
                    for t in range(GT):
                        zi = zctr[0] % 2
                        zctr[0] += 1
                        zt, zb = inproj(t, wbt, wbb, 512, zi)
                        Xt, Xb = X[t]
                        ds = slice(db * 512, (db + 1) * 512)
                        op(DVE, lambda: nc.vector.tensor_tensor(out=Xt[:, ds], in0=Xt[:, ds], in1=zt[:, 0:512], op=ALU.add), [Xb, zb], [Xb])
                for t in range(GT):
                    To = g * GT + t - NT_PRE
                    dma(x1s[To * 128:(To + 1) * 128, :], X[t][0][:], [X[t][1]], [x1b[To]], X[t][1])

        if STOP == 7:
            return nc
        with ExitStack() as pb:
            def sb(name, shape, dt=F32):
                return pb.enter_context(nc.sbuf_tensor(name, shape, dt)), Buf(name)

            def psb(name, shape, dt=F32):
                return pb.enter_context(nc.psum_tensor(name, shape, dt))

            Y = [sb("Y%d" % i, [128, D]) for i in range(TG)]
            h2T, h2Tb = sb("h2T", [128, 16, TG * 128], BF16)
            aT = [sb("aT%d" % i, [128, 4, max(TG * 128, 1024)], BF16) for i in range(1)]
            gst = [sb("gst%d" % i, [128, 4, 256]) for i in range(4)]
            gub = [sb("gub%d" % i, [128, 16, 256], BF16) for i in range(2)]
            dst_ = [sb("dst%d" % i, [128, 4, 512]) for i in range(2)]
            dnb = [sb("dnb%d" % i, [128, 4, 512], BF16) for i in range(2)]
            tmps = [[sb("at%d_%d" % (i, j), [128, 512]) for j in range(3)] for i in range(2)]
            normB, normBb = sb("normB", [128, D])
            bgu, bgub = sb("bgu", [128, NE * 2 * 16])
            Gt, Gtb = sb("G", [128, TG, NE])
            wrb, wrbb = sb("wrb", [128, 16, NE], BF16)
            bdnb, bdnbb = sb("bdnb", [NE, D], BF16)
            brB, brBb = sb("brB", [128, NE])
            hb, hbb = sb("hb2", [128, D], BF16)
            idb, idbb = sb("idb2", [128, 128], BF16)
            pT, pTb = sb("pT", [128, 2, TG * 128], BF16)
            lg, lgb = sb("lg", [128, NE]); m8, m8b = sb("m8", [128, 8]); msk, mskb = sb("msk", [128, NE])
            ex, exb = sb("ex", [128, NE]); Gb16, Gb16b = sb("Gb16", [128, NE], BF16); GTt, GTtb = sb("GTt", [NE, 128], BF16)
            r1, r1b = sb("r1", [128, 1]); r2, r2b = sb("r2", [128, 1]); r3, r3b = sb("r3", [128, 1]); r4, r4b = sb("r4", [128, 1])

            PG = [(psb("PG%d" % i, [128, 512]), Buf("PG%d" % i, True)) for i in range(2)]
            PU = [(psb("PU%d" % i, [128, 512]), Buf("PU%d" % i, True)) for i in range(2)]
            PY = [(psb("PY%d" % i, [128, 512]), Buf("PY%d" % i, True)) for i in range(2)]
            TR = psb("TR2", [128, 1024])
            TRb = [Buf("TR2a", True), Buf("TR2b", True)]

            op(POOL, lambda: nc.gpsimd.memset(idb[:], 1.0), [], [idbb])
            op(POOL, lambda: nc.gpsimd.affine_select(out=idb[:], in_=idb[:], pattern=[[-1, 128]], compare_op=ALU.is_equal,
                                                     fill=0.0, base=0, channel_multiplier=1), [idbb], [idbb])
            dma(bgu[:], bgu_t[:, :], [], [bgub], bgub)
            bguv = bgu[:].rearrange("p (e two c) -> p e two c", e=NE, two=2)
            op(DVE, lambda: nc.vector.tensor_scalar(out=bguv[:, :, 1, :], in0=bguv[:, :, 1, :], scalar1=1.0, scalar2=None, op0=ALU.add),
               [bgub], [bgub])
            dma(brB[:], b_r.partition_broadcast(128), [], [brBb], brBb)
            y0v = Y[0][0][:, 0:16 * NE].rearrange("p (c n) -> p c n", c=16)
            dma(y0v, w_r.rearrange("(c p) n -> p c n", p=128), [], [Y[0][1]], Y[0][1])
            op(POOL, lambda: nc.gpsimd.tensor_copy(out=wrb[:], in_=y0v), [Y[0][1]], [wrbb])
            dma(Y[1][0][0:NE, :], b_dn[:, :], [], [Y[1][1]], Y[1][1])
            op(POOL, lambda: nc.gpsimd.tensor_copy(out=bdnb[:], in_=Y[1][0][0:NE, :]), [Y[1][1]], [bdnbb])

            def rstd2(src, srcb, dst, dstb, n, eps):
                op(DVE, lambda: nc.vector.tensor_scalar(out=dst[:], in0=src[:], scalar1=1.0 / n, scalar2=eps,
                                                        op0=ALU.mult, op1=ALU.add), [srcb], [dstb])
                op(ACT, lambda: nc.scalar.activation(out=dst[:], in_=dst[:], func=AF.Ln), [dstb], [dstb])
                op(ACT, lambda: nc.scalar.activation(out=dst[:], in_=dst[:], func=AF.Exp, scale=-0.5), [dstb], [dstb])

            def norm_T(t, dstT, dstTb):
                Yt, Yb = Y[t]
                op(ACT, lambda: nc.scalar.activation(out=hb[:], in_=Yt[:], func=AF.Square, accum_out=r1[:]), [Yb], [hbb, r1b])
                rstd2(r1, r1b, r2, r2b, D, RMS_EPS)
                op(DVE, lambda: nc.vector.scalar_tensor_tensor(out=hb[:], in0=Yt[:], scalar=r2[:, 0:1], in1=normB[:],
                                                               op0=ALU.mult, op1=ALU.mult), [Yb, r2b, normBb], [hbb])
                for q4 in range(4):
                    trb = TRb[q4 % 2]
                    base = (q4 % 2) * 512
                    for j in range(4):
                        c = q4 * 4 + j
                        op(PE, lambda c=c, j=j: nc.tensor.matmul(TR[:, base + j * 128: base + (j + 1) * 128],
                                                                    hb[:, c * 128:(c + 1) * 128], idb[:], start=True, stop=True),
                           [hbb, idbb], [trb], mile=(j == 3))
                    srcv = TR[:, base:base + 512].rearrange("p (c t) -> p c t", c=4)
                    dstv = dstT[:, q4 * 4:(q4 + 1) * 4, t * 128:(t + 1) * 128]
                    if q4 % 2 == 0:
                        op(ACT, lambda: nc.scalar.copy(out=dstv, in_=srcv), [trb], [dstTb])
                    else:
                        op(DVE, lambda: nc.vector.tensor_copy(out=dstv, in_=srcv), [trb], [dstTb])

            gctr = {"gst": 0, "gub": 0, "dst": 0, "dnb": 0, "pg": 0, "py": 0, "tmp": 0, "aT": 0}

            def load_gu(src3, deint):
                slot = gctr["gub"] % 2
                gctr["gub"] += 1
                gt_, gb_ = gub[slot]
                for kh in range(4):
                    s = gctr["gst"] % 4
                    gctr["gst"] += 1
                    stt, stb = gst[s]
                    dma(stt[:], src3[kh * 512:(kh + 1) * 512, :].rearrange("(c p) n -> p c n", p=128), [], [stb], stb)
                    if deint:
                        iv = stt[:].rearrange("p c (f two) -> p c two f", two=2)
                        ov = gt_[:, kh * 4:(kh + 1) * 4, :].rearrange("p c (two f) -> p c two f", two=2)
                    else:
                        iv = stt[:]
                        ov = gt_[:, kh * 4:(kh + 1) * 4, :]
                    op(ACT, lambda: nc.scalar.copy(out=ov, in_=iv), [stb], [gb_])
                return gt_, gb_

            def load_dn(e, qt, db):
                slot = gctr["dnb"] % 2
                gctr["dnb"] += 1
                s = gctr["dst"] % 2
                gctr["dst"] += 1
                stt, stb = dst_[s]
                dt_, db_ = dnb[slot]
                dma(stt[:], w_dn[e, qt * 512:(qt + 1) * 512, db * 512:(db + 1) * 512].rearrange("(c p) n -> p c n", p=128),
                    [], [stb], stb)
                op(POOL, lambda: nc.gpsimd.tensor_copy(out=dt_[:], in_=stt[:]), [stb], [db_])
                return dt_, db_

            for grp in range(NT_OWN // TG):
                dma(normB[:], norm_ffn.partition_broadcast(128), [], [normBb], normBb)
                for t in range(TG):
                    To = grp * TG + t
                    Yt, Yb = Y[t]
                    dma(Yt[:], x1s[To * 128:(To + 1) * 128, :], [x1b[To]], [Yb], Yb)
                    norm_T(t, h2T, h2Tb)
                    ts_ = slice(t * 128, (t + 1) * 128)
                    PL, PLb = PY[gctr["py"] % 2]
                    gctr["py"] += 1
                    for c in range(16):
                        op(PE, lambda c=c: nc.tensor.matmul(PL[:, 0:NE], h2T[:, c, ts_], wrb[:, c, :], start=(c == 0), stop=(c == 15)),
                           [h2Tb, wrbb], [PLb], mile=(c == 15))
                    op(DVE, lambda: nc.vector.tensor_tensor(out=lg[:], in0=PL[:, 0:NE], in1=brB[:], op=ALU.add), [PLb, brBb], [lgb])
                    op(DVE, lambda: nc.vector.max(out=m8[:], in_=lg[:]), [lgb], [m8b])
                    op(DVE, lambda: nc.vector.tensor_scalar(out=msk[:], in0=lg[:], scalar1=m8[:, 3:4], scalar2=None, op0=ALU.is_ge),
                       [lgb, m8b], [mskb])
                    op(DVE, lambda: nc.vector.tensor_scalar(out=r3[:], in0=m8[:, 0:1], scalar1=-1.0, scalar2=None, op0=ALU.mult),
                       [m8b], [r3b])
                    op(ACT, lambda: nc.scalar.activation(out=ex[:], in_=lg[:], func=AF.Exp, bias=r3[:, 0:1], scale=1.0), [lgb, r3b], [exb])
                    op(DVE, lambda: nc.vector.tensor_tensor(out=ex[:], in0=ex[:], in1=msk[:], op=ALU.mult), [exb, mskb], [exb])
                    op(DVE, lambda: nc.vector.reduce_sum(out=r4[:], in_=ex[:], axis=AX.X), [exb], [r4b])
                    op(ACT, lambda: nc.scalar.activation(out=r4[:], in_=r4[:], func=AF.Ln), [r4b], [r4b])
                    op(DVE, lambda: nc.vector.tensor_tensor(out=r3[:], in0=r3[:], in1=r4[:], op=ALU.subtract), [r3b, r4b], [r3b])
                    op(ACT, lambda: nc.scalar.activation(out=ex[:], in_=lg[:], func=AF.Exp, bias=r3[:, 0:1], scale=1.0), [lgb, r3b], [exb])
                    op(DVE, lambda: nc.vector.tensor_tensor(out=Gt[:, t, :], in0=ex[:], in1=msk[:], op=ALU.mult), [exb, mskb], [Gtb])
                    op(POOL, lambda: nc.gpsimd.tensor_copy(out=Gb16[:], in_=Gt[:, t, :]), [Gtb], [Gb16b])
                    op(PE, lambda: nc.tensor.matmul(TR[0:NE, 0:128], Gb16[:], idb[:], start=True, stop=True), [Gb16b, idbb], [TRb[0]])
                    op(ACT, lambda: nc.scalar.copy(out=GTt[:], in_=TR[0:NE, 0:128]), [TRb[0]], [GTtb])
                    for db in range(4):
                        ds = slice(db * 512, (db + 1) * 512)
                        pyt, pyb = PY[gctr["py"] % 2]
                        gctr["py"] += 1
                        op(PE, lambda: nc.tensor.matmul(pyt[:], GTt[:], bdnb[:, ds], start=True, stop=True), [GTtb, bdnbb], [pyb])
                        op(DVE, lambda: nc.vector.tensor_tensor(out=Yt[:, ds], in0=Yt[:, ds], in1=pyt[:], op=ALU.add), [Yb, pyb], [Yb])

                if STOP == 8:
                    return nc
                def gu_compute(e, fc, gt_, gb_, at, atb, j):
                    for tg in range(TG // 4):
                        tsl = slice(tg * 512, (tg + 1) * 512)
                        pi = gctr["pg"] % 2
                        gctr["pg"] += 1
                        pgt, pgb = PG[pi]
                        put, pub = PU[pi]
                        for c in range(16):
                            op(PE, lambda c=c: nc.tensor.matmul(pgt[:], gt_[:, c, 0:128], h2T[:, c, tsl], start=(c == 0), stop=(c == 15)),
                               [gb_, h2Tb], [pgb], mile=(c == 15))
                        for c in range(16):
                            op(PE, lambda c=c: nc.tensor.matmul(put[:], gt_[:, c, 128:256], h2T[:, c, tsl], start=(c == 0), stop=(c == 15)),
                               [gb_, h2Tb], [pub], mile=(c == 15))
                        tm = tmps[gctr["tmp"] % 2]
                        gctr["tmp"] += 1
                        (gc, gcb), (sg, sgb), (xx, xxb) = tm
                        ig = (e * 2 + 0) * 16 + fc
                        iu = (e * 2 + 1) * 16 + fc
                        op(DVE, lambda: nc.vector.tensor_scalar(out=gc[:], in0=pgt[:], scalar1=bgu[:, ig:ig + 1], scalar2=7.0,
                                                                op0=ALU.add, op1=ALU.min), [pgb, bgub], [gcb])
                        op(ACT, lambda: nc.scalar.activation(out=sg[:], in_=gc[:], func=AF.Sigmoid, scale=1.702), [gcb], [sgb])
                        op(DVE, lambda: nc.vector.tensor_scalar(out=xx[:], in0=put[:], scalar1=bgu[:, iu:iu + 1], scalar2=-6.0,
                                                                op0=ALU.add, op1=ALU.max), [pub, bgub], [xxb])
                        op(POOL, lambda: nc.gpsimd.tensor_tensor(out=gc[:], in0=gc[:], in1=sg[:], op=ALU.mult), [gcb, sgb], [gcb])
                        op(DVE, lambda: nc.vector.scalar_tensor_tensor(out=at[:, j, tsl], in0=xx[:], scalar=8.0, in1=gc[:],
                                                                       op0=ALU.min, op1=ALU.mult), [xxb, gcb], [atb])

                def dn_compute(e, dt_, db_, at, atb, db):
                    ds = slice(db * 512, (db + 1) * 512)
                    for t in range(TG):
                        ts_ = slice(t * 128, (t + 1) * 128)
                        pyt, pyb = PY[gctr["py"] % 2]
                        gctr["py"] += 1
                        for j in range(4):
                            op(PE, lambda j=j: nc.tensor.matmul(pyt[:], at[:, j, ts_], dt_[:, j, :], start=(j == 0), stop=(j == 3)),
                               [atb, db_], [pyb], mile=(j == 3))
                        Yt, Yb = Y[t]
                        op(DVE, lambda: nc.vector.scalar_tensor_tensor(out=Yt[:, ds], in0=pyt[:], scalar=Gt[:, t, e:e + 1], in1=Yt[:, ds],
                                                                       op0=ALU.mult, op1=ALU.add), [pyb, Gtb, Yb], [Yb])

                steps = []
                for e in range(NE):
                    for qt in range(4):
                        for j in range(4):
                            steps.append(("gu", e, qt, j))
                        for db in range(4):
                            steps.append(("dn", e, qt, db))

                def issue_load(st):
                    kind, e, qt, j = st
                    if kind == "gu":
                        fc = qt * 4 + j
                        return load_gu(w_gu[e, :, fc * 256:(fc + 1) * 256], True)
                    return load_dn(e, qt, j)

                pend = {0: issue_load(steps[0])}
                cur_at = None
                for si, st in enumerate(steps):
                    if si + 1 < len(steps):
                        pend[si + 1] = issue_load(steps[si + 1])
                    wt_, wb_ = pend.pop(si)
                    kind, e, qt, j = st
                    if kind == "gu":
                        if j == 0:
                            cur_at = aT[0]
                        gu_compute(e, qt * 4 + j, wt_, wb_, cur_at[0], cur_at[1], j)
                    else:
                        dn_compute(e, wt_, wb_, cur_at[0], cur_at[1], j)

                if STOP == 9:
                    return nc
                dma(normB[:], norm_ple.partition_broadcast(128), [], [normBb], normBb)
                wpp_t, wpp_b = aT[0]
                wppv = wpp_t[:].rearrange("p a b -> p (a b)")[:, 0:4096].rearrange("p (c n) -> p c n", c=2)
                for c2 in range(2):
                    stt, stb = gst[c2]
                    for hh in range(2):
                        sv = stt[:].rearrange("p a b -> p (a b)")
                        dma(sv, w_pp[c2 * 128:(c2 + 1) * 128, hh * 1024:(hh + 1) * 1024], [], [stb], stb)
                        op(POOL, lambda: nc.gpsimd.tensor_copy(out=wppv[:, c2, hh * 1024:(hh + 1) * 1024], in_=sv), [stb], [wpp_b])
                for t in range(TG):
                    To = grp * TG + t
                    norm_T(t, h2T, h2Tb)
                    pt, ptb = tmps[0][0]
                    dma(pt[:, 0:256], pin[To * 128:(To + 1) * 128, :], [], [ptb], ptb)
                    op(POOL, lambda: nc.gpsimd.tensor_copy(out=hb[:, 0:256], in_=pt[:, 0:256]), [ptb], [hbb])
                    for c2 in range(2):
                        op(PE, lambda: nc.tensor.matmul(TR[:, c2 * 128:(c2 + 1) * 128], hb[:, c2 * 128:(c2 + 1) * 128], idb[:], start=True, stop=True),
                           [hbb, idbb], [TRb[0]], mile=(c2 == 1))
                    op(DVE, lambda: nc.vector.tensor_copy(out=pT[:, :, t * 128:(t + 1) * 128],
                                                          in_=TR[:, 0:256].rearrange("p (c t) -> p c t", c=2)), [TRb[0]], [pTb])
                for gb8 in range(8):
                    gt_, gb_ = load_gu(w_pg[:, gb8 * 256:(gb8 + 1) * 256], False)
                    cs = slice(gb8 * 256, (gb8 + 1) * 256)
                    for t in range(TG):
                        ts_ = slice(t * 128, (t + 1) * 128)
                        pi = gctr["pg"] % 2
                        gctr["pg"] += 1
                        pgt, pgb = PG[pi]
                        put, pub = PU[pi]
                        for c in range(16):
                            op(PE, lambda c=c: nc.tensor.matmul(pgt[:, 0:256], h2T[:, c, ts_], gt_[:, c, :], start=(c == 0), stop=(c == 15)),
                               [h2Tb, gb_], [pgb], mile=(c == 15))
                        for c2 in range(2):
                            op(PE, lambda c2=c2: nc.tensor.matmul(put[:, 0:256], pT[:, c2, ts_], wppv[:, c2, cs], start=(c2 == 0), stop=(c2 == 1)),
                               [pTb, wpp_b], [pub], mile=(c2 == 1))
                        tm = tmps[gctr["tmp"] % 2]
                        gctr["tmp"] += 1
                        (gc, gcb), (sg, sgb) = tm[0], tm[1]
                        Yt, Yb = Y[t]
                        op(ACT, lambda: nc.scalar.activation(out=gc[:, 0:256], in_=pgt[:, 0:256], func=AF.Sigmoid), [pgb], [gcb])
                        op(DVE, lambda: nc.vector.tensor_tensor(out=sg[:, 0:256], in0=gc[:, 0:256], in1=put[:, 0:256], op=ALU.mult),
                           [gcb, pub], [sgb])
                        op(DVE, lambda: nc.vector.tensor_tensor(out=Yt[:, cs], in0=Yt[:, cs], in1=sg[:, 0:256], op=ALU.add), [Yb, sgb], [Yb])
                dma(normB[:], norm_final.partition_broadcast(128), [], [normBb], normBb)
                for t in range(TG):
                    To = grp * TG + t
                    Yt, Yb = Y[t]
                    op(ACT, lambda: nc.scalar.activation(out=hb[:], in_=Yt[:], func=AF.Square, accum_out=r1[:]), [Yb], [hbb, r1b])
                    rstd2(r1, r1b, r2, r2b, D, RMS_EPS)
                    op(DVE, lambda: nc.vector.scalar_tensor_tensor(out=Yt[:], in0=Yt[:], scalar=r2[:, 0:1], in1=normB[:],
                                                                   op0=ALU.mult, op1=ALU.mult), [Yb, r2b, normBb], [Yb])
                    ob = Buf("o%d" % To)
                    out_toks.append(dma(out[To * 128:(To + 1) * 128, :], Yt[:], [Yb], [ob], Yb))
            for tok in out_toks:
                SP.wait(tok)
    return nc


def _layouts(inputs):
    f = lambda a: np.ascontiguousarray(np.asarray(a, dtype=np.float32))
    x = f(inputs["x"]); p = f(inputs["p"])
    shared = {
        "w_in": f(inputs["w_in"][0]), "w_out": f(inputs["w_out"][0]),
        "w_gu": f(inputs["w_gate_up"][0]), "w_dn": f(inputs["w_down"][0]),
        "w_pg": f(inputs["w_ple_gate"][0]), "w_pp": f(inputs["w_ple_proj"][0]),
        "w_r": f(inputs["w_router"][0]),
        "norm_mix": f(inputs["norm_mix"][0]), "norm_ffn": f(inputs["norm_ffn"][0]),
        "norm_ple": f(inputs["norm_ple"][0]), "norm_final": f(inputs["norm_final"]),
        "lb_logits": f(inputs["lb_logits"]),
        "hon_t": f(np.asarray(inputs["hgrn_out_norm"][0]).reshape(8, 128).T),
        "ln_g": f(inputs["gmlp_ln_g"][0]), "ln_b": f(inputs["gmlp_ln_b"][0]),
        "ws_t": f(np.asarray(inputs["w_spatial"][0]).transpose(2, 0, 1)),
        "bs_t": f(np.asarray(inputs["b_spatial"][0]).T),
        "b_r": f(inputs["b_router"][0]),
        "bgu_t": f(np.asarray(inputs["b_gate_up"][0]).reshape(NE, 16, 128, 2).transpose(2, 0, 3, 1).reshape(128, NE * 2 * 16)),
        "b_dn": f(inputs["b_down"][0]),
    }
    maps = []
    for c in range(8):
        b, half = c // 2, c % 2
        own = x[b, half * 2048:(half + 1) * 2048]
        pre = x[b, 0:2048] if half == 1 else np.zeros_like(own)
        m = dict(shared)
        m["xs"] = np.ascontiguousarray(np.concatenate([pre, own], axis=0))
        m["p"] = np.ascontiguousarray(p[0, b, half * 2048:(half + 1) * 2048])
        maps.append(m)
    return maps


def kernel(**inputs):
    maps = _layouts(inputs)
    nc = build_nc()
    res = run_bass_kernel_spmd(nc, maps, core_ids=list(range(8)))
    outp = np.empty((4, 4096, 2048), np.float32)
    for c in range(8):
        b, half = c // 2, c % 2
        outp[b, half * 2048:(half + 1) * 2048] = np.asarray(res.results[c]["out"], dtype=np.float32)
    return outp
```

```python
import numpy as np
from contextlib import ExitStack
import concourse.bass as bass
import concourse.mybir as mybir
from concourse.bass_utils import run_bass_kernel_spmd

F32 = mybir.dt.float32
BF16 = mybir.dt.bfloat16
AF = mybir.ActivationFunctionType
ALU = mybir.AluOpType
AX = mybir.AxisListType

D = 2048
NT_OWN = 16
NT_PRE = 16
GT = 4
TG = 8
NE = 32
RMS_EPS = 1e-6
LN_EPS = 1e-5
DEBUG = False
STOP = 0
SUB = 0


class Eng:
    def __init__(self, e, sem, is_pe=False):
        self.e = e
        self.sem = sem
        self.cnt = 0
        self.seen = {}
        self.is_pe = is_pe

    def wait(self, tok):
        if tok is None:
            return
        sem, val = tok
        if sem is self.sem and self.is_pe:
            return
        key = id(sem)
        if self.seen.get(key, 0) >= val:
            return
        self.e.wait_ge(sem, val)
        self.seen[key] = val

    def done(self, ins):
        ins.then_inc(self.sem, 1)
        self.cnt += 1
        return (self.sem, self.cnt)

    def future(self):
        return (self.sem, self.cnt + 1)


class Buf:
    def __init__(self, name="", excl=False):
        self.name = name
        self.excl = excl
        self.w = None
        self.r = {}
        self.dsem = None
        self.dcnt = 0


def _deps(E, reads, writes):
    for b in reads:
        E.wait(b.w)
        if b.excl:
            for t in list(b.r.values()):
                if t[0] is not E.sem:
                    E.wait(t)
    for b in writes:
        E.wait(b.w)
        for t in list(b.r.values()):
            E.wait(t)


def _upd(tok, reads, writes):
    for b in reads:
        k = id(tok[0])
        if k not in b.r or b.r[k][1] < tok[1]:
            b.r[k] = tok
    for b in writes:
        b.w = tok
        b.r = {}


def op(E, fn, reads=(), writes=(), mile=True):
    _deps(E, reads, writes)
    ins = fn()
    tok = E.done(ins) if mile else E.future()
    _upd(tok, reads, writes)
    return tok


def build_nc():
    nc = bass.Bass("TRN2", target_bir_lowering=False)

    def din(name, shape):
        return nc.dram_tensor(name, shape, F32, kind="ExternalInput").ap()

    xs = din("xs", [(NT_PRE + NT_OWN) * 128, D])
    pin = din("p", [NT_OWN * 128, 256])
    w_in = din("w_in", [D, 6144])
    w_out = din("w_out", [D, D])
    w_gu = din("w_gu", [NE, D, 4096])
    w_dn = din("w_dn", [NE, D, D])
    w_pg = din("w_pg", [D, D])
    w_pp = din("w_pp", [256, D])
    w_r = din("w_r", [D, NE])
    norm_mix = din("norm_mix", [D])
    norm_ffn = din("norm_ffn", [D])
    norm_ple = din("norm_ple", [D])
    norm_final = din("norm_final", [D])
    lb_logits = din("lb_logits", [2, 1024])
    hon_t = din("hon_t", [128, 8])
    ln_g = din("ln_g", [1024])
    ln_b = din("ln_b", [1024])
    ws_t = din("ws_t", [128, 8, 128])
    bs_t = din("bs_t", [128, 8])
    b_r = din("b_r", [NE])
    bgu_t = din("bgu_t", [128, NE * 2 * 16])
    b_dn = din("b_dn", [NE, D])
    out = nc.dram_tensor("out", [NT_OWN * 128, D], F32, kind="ExternalOutput").ap()
    x1s = nc.dram_tensor("x1s", [NT_OWN * 128, D], F32, kind=("ExternalOutput" if DEBUG else "Internal")).ap()

    es = ExitStack()
    with es:
        def sem(name):
            return es.enter_context(nc.semaphore(name))

        PE = Eng(nc.tensor, sem("s_pe"), is_pe=True)
        ACT = Eng(nc.scalar, sem("s_act"))
        DVE = Eng(nc.vector, sem("s_dve"))
        POOL = Eng(nc.gpsimd, sem("s_pool"))
        SP = Eng(nc.sync, sem("s_sp"))

        def dma(out_ap, in_ap, reads, writes, sb):
            if sb.dsem is None:
                sb.dsem = sem("d_" + sb.name)
            _deps(SP, reads, writes)
            SP.e.dma_start(out=out_ap, in_=in_ap).then_inc(sb.dsem, 16)
            sb.dcnt += 16
            tok = (sb.dsem, sb.dcnt)
            _upd(tok, reads, writes)
            return tok

        x1b = [Buf("x1s%d" % i) for i in range(NT_OWN)]
        out_toks = []

        with ExitStack() as pa:
            def sb(name, shape, dt=F32):
                return pa.enter_context(nc.sbuf_tensor(name, shape, dt)), Buf(name)

            def psb(name, shape, dt=F32):
                return pa.enter_context(nc.psum_tensor(name, shape, dt))

            X = [sb("X%d" % i, [128, D]) for i in range(GT)]
            hT, hTb = sb("hT", [128, 16, GT * 128], BF16)
            hb, hbb = sb("hb", [128, D], BF16)
            stg = [sb("stg%d" % i, [128, 16, 128]) for i in range(3)]
            wb = [sb("wb%d" % i, [128, 16, 512], BF16) for i in range(2)]
            mix = [sb("mix%d" % i, [128, D], BF16) for i in range(GT)]
            GU = [sb("GU%d" % i, [128, 1024]) for i in range(GT)]
            GV = [sb("GV%d" % i, [128, 1024]) for i in range(GT)]
            vn, vnb = sb("vn", [128, 1024], BF16)
            nmB, nmBb = sb("nmB", [128, D])
            lbB, lbBb = sb("lbB", [128, 1024])
            omlB, omlBb = sb("omlB", [128, 1024])
            lngB, lngBb = sb("lngB", [128, 1024])
            lnbB, lnbBb = sb("lnbB", [128, 1024])
            wsT, wsTb = sb("wsT", [128, 8, 128], BF16)
            bsT, bsTb = sb("bsT", [128, 8])
            hon, honb = sb("hon", [128, 8])
            idb, idbb = sb("idb", [128, 128], BF16)
            triF, triFb = sb("triF", [128, 128])
            ind, indb = sb("ind", [128, 2])
            S = [sb("S%d" % i, [128, 128]) for i in range(8)]
            Sb = [sb("Sb%d" % i, [128, 128], BF16) for i in range(8)]
            def mkset(i):
                d = {}
                for nm in ["sf", "sq", "sgt", "ff", "lf", "kk", "enb", "eb", "qs", "tmpS"]:
                    d[nm] = sb("%s_%d" % (nm, i), [128, 128])
                d["dec"] = sb("dec_%d" % i, [128, 2])
                for nm in ["vbt", "ke", "qe", "keT", "qeT", "scm"]:
                    d[nm] = sb("%s_%d" % (nm, i), [128, 128], BF16)
                d["u3"] = sb("u3_%d" % i, [128, 1])
                d["u4"] = sb("u4_%d" % i, [128, 1])
                return d
            TS = [mkset(0), mkset(1)]
            uctr = [0]
            st1, st1b = sb("st1", [128, 1]); st2, st2b = sb("st2", [128, 1]); st3, st3b = sb("st3", [128, 1])
            st4, st4b = sb("st4", [128, 1]); st5, st5b = sb("st5", [128, 1])

            Z = [(psb("Z%d" % i, [128, 512]), Buf("Z%d" % i, True)) for i in range(2)]
            UA = [psb("UA%d" % i, [128, 512]) for i in range(2)]
            UAb = [Buf("UA%d" % i, True) for i in range(2)]
            UB = [psb("UB%d" % i, [128, 512]) for i in range(2)]
            UBb = [Buf("UB%d" % i, True) for i in range(2)]
            SPa = Buf("SPa", True)
            SPbk = Buf("SPbk", True)
            SPp = psb("SPp", [128, 1024])

            cst = Buf("cst")

            dma(nmB[:], norm_mix.partition_broadcast(128), [], [nmBb], nmBb)
            dma(lbB[:], lb_logits[0, :].partition_broadcast(128), [], [lbBb], lbBb)
            dma(omlB[:], lb_logits[1, :].partition_broadcast(128), [], [omlBb], omlBb)
            dma(lngB[:], ln_g.partition_broadcast(128), [], [lngBb], lngBb)
            dma(lnbB[:], ln_b.partition_broadcast(128), [], [lnbBb], lnbBb)
            dma(bsT[:], bs_t[:, :], [], [bsTb], bsTb)
            dma(hon[:], hon_t[:, :], [], [honb], honb)
            gu0v = GU[0][0][:].rearrange("p (g t) -> p g t", g=8)
            dma(gu0v, ws_t[:, :, :], [], [GU[0][1]], GU[0][1])
            op(POOL, lambda: nc.gpsimd.affine_select(out=gu0v, in_=gu0v, pattern=[[0, 8], [1, 128]],
                                                     compare_op=ALU.is_ge, fill=0.0, base=0, channel_multiplier=-1),
               [GU[0][1]], [GU[0][1]])
            op(POOL, lambda: nc.gpsimd.tensor_copy(out=wsT[:], in_=gu0v), [GU[0][1]], [wsTb])
            op(POOL, lambda: nc.gpsimd.memset(idb[:], 1.0), [], [idbb])
            op(POOL, lambda: nc.gpsimd.affine_select(out=idb[:], in_=idb[:], pattern=[[-1, 128]], compare_op=ALU.is_equal,
                                                     fill=0.0, base=0, channel_multiplier=1), [idbb], [idbb])
            op(POOL, lambda: nc.gpsimd.memset(triF[:], 1.0), [], [triFb])
            op(POOL, lambda: nc.gpsimd.affine_select(out=triF[:], in_=triF[:], pattern=[[1, 128]], compare_op=ALU.is_ge,
                                                     fill=0.0, base=0, channel_multiplier=-1), [triFb], [triFb])
            op(POOL, lambda: nc.gpsimd.memset(triF[0:64, 64:128], 0.0), [triFb], [triFb])
            op(POOL, lambda: nc.gpsimd.memset(ind[:], 0.0), [], [indb])
            op(POOL, lambda: nc.gpsimd.memset(ind[0:64, 0:1], 1.0), [indb], [indb])
            op(POOL, lambda: nc.gpsimd.memset(ind[64:128, 1:2], 1.0), [indb], [indb])
            for h in range(8):
                op(POOL, lambda h=h: nc.gpsimd.memset(S[h][0][:], 0.0), [], [S[h][1]])
                op(POOL, lambda h=h: nc.gpsimd.memset(Sb[h][0][:], 0.0), [], [Sb[h][1]])
            op(DVE, lambda: nc.vector.tensor_tensor(out=lbB[:], in0=lbB[:], in1=omlB[:], op=ALU.subtract), [lbBb, omlBb], [lbBb])
            op(ACT, lambda: nc.scalar.activation(out=lbB[:], in_=lbB[:], func=AF.Sigmoid), [lbBb], [lbBb])
            op(DVE, lambda: nc.vector.tensor_scalar(out=omlB[:], in0=lbB[:], scalar1=-1.0, scalar2=1.0, op0=ALU.mult, op1=ALU.add),
               [lbBb], [omlBb])

            if STOP == 1:
                return nc

            def rstd_from_sum(src, srcb, dst, dstb, n, eps):
                op(DVE, lambda: nc.vector.tensor_scalar(out=dst[:], in0=src[:], scalar1=1.0 / n, scalar2=eps,
                                                        op0=ALU.mult, op1=ALU.add), [srcb], [dstb])
                op(ACT, lambda: nc.scalar.activation(out=dst[:], in_=dst[:], func=AF.Ln), [dstb], [dstb])
                op(ACT, lambda: nc.scalar.activation(out=dst[:], in_=dst[:], func=AF.Exp, scale=-0.5), [dstb], [dstb])

            def transpose16(src, srcb, dstT, dstTb, tcol):
                for q4 in range(4):
                    TRt, trb = UB[q4 % 2], UBb[q4 % 2]
                    for j in range(4):
                        c = q4 * 4 + j
                        op(PE, lambda c=c, j=j: nc.tensor.matmul(TRt[:, j * 128:(j + 1) * 128],
                                                                 src[:, c * 128:(c + 1) * 128], idb[:], start=True, stop=True),
                           [srcb, idbb], [trb], mile=(j == 3))
                    srcv = TRt[:, 0:512].rearrange("p (c t) -> p c t", c=4)
                    dstv = dstT[:, q4 * 4:(q4 + 1) * 4, tcol:tcol + 128]
                    if q4 % 2 == 0:
                        op(ACT, lambda: nc.scalar.copy(out=dstv, in_=srcv), [trb], [dstTb])
                    else:
                        op(DVE, lambda: nc.vector.tensor_copy(out=dstv, in_=srcv), [trb], [dstTb])

            wstate = {"stg": 0, "wb": 0}

            def load_block(wmat, cols, fold_hon=False):
                slot = wstate["wb"] % 2
                wstate["wb"] += 1
                wbt, wbb = wb[slot]
                for j, c0 in enumerate(cols):
                    s = wstate["stg"] % 3
                    wstate["stg"] += 1
                    stt, stb = stg[s]
                    dma(stt[:], wmat[:, c0:c0 + 128].rearrange("(c p) n -> p c n", p=128), [], [stb], stb)
                    if fold_hon:
                        honv = hon[:, 0:8].rearrange("p (c o) -> p c o", o=1).to_broadcast([128, 8, 128])
                        op(POOL, lambda: nc.gpsimd.tensor_tensor(out=wbt[:, 0:8, j * 128:(j + 1) * 128], in0=stt[:, 0:8, :],
                                                                 in1=honv, op=ALU.mult), [stb, honb], [wbb])
                        op(POOL, lambda: nc.gpsimd.tensor_copy(out=wbt[:, 8:16, j * 128:(j + 1) * 128], in_=stt[:, 8:16, :]),
                           [stb], [wbb])
                    else:
                        op(POOL, lambda: nc.gpsimd.tensor_copy(out=wbt[:, :, j * 128:(j + 1) * 128], in_=stt[:, :, :]),
                           [stb], [wbb])
                return wbt, wbb

            def inproj(t, wbt, wbb, ncols, zi):
                zt, zb = Z[zi]
                for c in range(16):
                    op(PE, lambda c=c: nc.tensor.matmul(zt[:, 0:ncols], hT[:, c, t * 128:(t + 1) * 128], wbt[:, c, 0:ncols],
                                                        start=(c == 0), stop=(c == 15)),
                       [hTb, wbb], [zb], mile=(c == 15))
                return zt, zb

            def hgrn_unit(t, hd, own, zb, zq, zf, zv, zg):
                hs = slice(hd * 128, (hd + 1) * 128)
                tset = TS[uctr[0] % 2]
                uctr[0] += 1
                (sf, sfb), (sq, sqb), (sgt, sgtb), (ff, ffb), (lf, lfb), (kk, kkb) = (tset[k_] for k_ in ["sf", "sq", "sgt", "ff", "lf", "kk"])
                (enb, enbb), (eb, ebb), (qs, qsb), (tmpS, tmpSb), (dec, decb) = (tset[k_] for k_ in ["enb", "eb", "qs", "tmpS", "dec"])
                (vbt, vbtb), (ke, keb), (qe, qeb), (keT, keTb), (qeT, qeTb), (scm, scmb) = (tset[k_] for k_ in ["vbt", "ke", "qe", "keT", "qeT", "scm"])
                (u3, u3b), (u4, u4b) = tset["u3"], tset["u4"]
                u_ = (uctr[0] - 1) % 2
                UAt, UAk, UBt, UBk = UA[u_], UAb[u_], UB[u_], UBb[u_]
                op(ACT, lambda: nc.scalar.activation(out=sf[:], in_=zf, func=AF.Sigmoid), [zb], [sfb])
                if own:
                    op(ACT, lambda: nc.scalar.activation(out=sq[:], in_=zq, func=AF.Sigmoid), [zb], [sqb])
                    op(ACT, lambda: nc.scalar.activation(out=sgt[:], in_=zg, func=AF.Sigmoid), [zb], [sgtb])
                op(ACT, lambda: nc.scalar.copy(out=vbt[:], in_=zv), [zb], [vbtb])
                op(DVE, lambda: nc.vector.tensor_tensor(out=ff[:], in0=sf[:], in1=omlB[:, hs], op=ALU.mult), [sfb, omlBb], [ffb])
                op(DVE, lambda: nc.vector.tensor_tensor(out=ff[:], in0=ff[:], in1=lbB[:, hs], op=ALU.add), [ffb, lbBb], [ffb])
                op(ACT, lambda: nc.scalar.activation(out=lf[:], in_=ff[:], func=AF.Ln), [ffb], [lfb])
                op(POOL, lambda: nc.gpsimd.tensor_scalar(out=kk[:], in0=ff[:], scalar1=-1.0, scalar2=1.0, op0=ALU.mult, op1=ALU.add),
                   [ffb], [kkb])
                if own:
                    op(DVE, lambda: nc.vector.tensor_tensor(out=qs[:], in0=zq, in1=sq[:], op=ALU.mult), [zb, sqb], [qsb])
                op(PE, lambda: nc.tensor.matmul(UAt[:, 0:128], triF[:], lf[:], start=True, stop=True), [triFb, lfb], [UAk])
                op(PE, lambda: nc.tensor.matmul(UAt[:, 384:386], lf[:], ind[:], start=True, stop=True), [lfb, indb], [UAk])
                op(ACT, lambda: nc.scalar.activation(out=enb[:], in_=UAt[:, 0:128], func=AF.Exp, scale=-1.0), [UAk], [enbb])
                if own:
                    op(ACT, lambda: nc.scalar.activation(out=eb[:], in_=UAt[:, 0:128], func=AF.Exp), [UAk], [ebb])
                op(ACT, lambda: nc.scalar.activation(out=dec[:], in_=UAt[:, 384:386], func=AF.Exp), [UAk], [decb])
                op(DVE, lambda: nc.vector.tensor_tensor(out=ke[:], in0=kk[:], in1=enb[:], op=ALU.mult), [kkb, enbb], [keb])
                if own:
                    op(DVE, lambda: nc.vector.tensor_tensor(out=qe[:], in0=qs[:], in1=eb[:], op=ALU.mult), [qsb, ebb], [qeb])
                    op(PE, lambda: nc.tensor.matmul(UBt[:, 0:128], ke[:], idb[:], start=True, stop=True), [keb, idbb], [UBk])
                    op(PE, lambda: nc.tensor.matmul(UBt[:, 128:256], qe[:], idb[:], start=True, stop=True), [qeb, idbb], [UBk])
                    op(ACT, lambda: nc.scalar.copy(out=keT[:], in_=UBt[:, 0:128]), [UBk], [keTb])
                    op(DVE, lambda: nc.vector.tensor_copy(out=qeT[:], in_=UBt[:, 128:256]), [UBk], [qeTb])
                    op(PE, lambda: nc.tensor.matmul(UAt[:, 128:256], keT[:], qeT[:], start=True, stop=True), [keTb, qeTb], [UAk])
                    op(DVE, lambda: nc.vector.tensor_tensor(out=scm[:], in0=UAt[:, 128:256], in1=triF[:], op=ALU.mult),
                       [UAk, triFb], [scmb])
                St, Stb = S[hd]
                Sbt, Sbb = Sb[hd]
                for ch in range(2):
                    rs = slice(ch * 64, (ch + 1) * 64)
                    if own:
                        op(PE, lambda: nc.tensor.matmul(UAt[rs, 256:384], scm[rs, rs], vbt[rs, :], start=True, stop=False),
                           [scmb, vbtb], [UAk], mile=False)
                        op(PE, lambda: nc.tensor.matmul(UAt[rs, 256:384], qeT[:, rs], Sbt[:], start=False, stop=True),
                           [qeTb, Sbb], [UAk])
                    su = UBt[:, 256 + ch * 128:256 + (ch + 1) * 128]
                    op(PE, lambda: nc.tensor.matmul(su, ke[rs, :], vbt[rs, :], start=True, stop=True), [keb, vbtb], [UBk])
                    op(DVE, lambda: nc.vector.tensor_tensor(out=tmpS[:], in0=su, in1=St[:], op=ALU.add), [UBk, Stb], [tmpSb])
                    op(DVE, lambda: nc.vector.tensor_scalar(out=St[:], in0=tmpS[:], scalar1=dec[:, ch:ch + 1], scalar2=None, op0=ALU.mult),
                       [tmpSb, decb], [Stb])
                    op(POOL, lambda: nc.gpsimd.tensor_copy(out=Sbt[:], in_=St[:]), [Stb], [Sbb])
                if own:
                    op(ACT, lambda: nc.scalar.activation(out=qs[:], in_=UAt[:, 256:384], func=AF.Square, accum_out=u3[:]),
                       [UAk], [qsb, u3b])
                    rstd_from_sum(u3, u3b, u4, u4b, 128, RMS_EPS)
                    op(DVE, lambda: nc.vector.scalar_tensor_tensor(out=mix[t][0][:, hs], in0=UAt[:, 256:384], scalar=u4[:, 0:1],
                                                                   in1=sgt[:], op0=ALU.mult, op1=ALU.mult),
                       [UAk, u4b, sgtb], [mix[t][1]])
            zctr = [0]
            for g in range((NT_PRE + NT_OWN) // GT):
                own = g >= NT_PRE // GT
                for t in range(GT):
                    Xt, Xb = X[t]
                    T = g * GT + t
                    dma(Xt[:], xs[T * 128:(T + 1) * 128, :], [], [Xb], Xb)
                    op(ACT, lambda: nc.scalar.activation(out=hb[:], in_=Xt[:], func=AF.Square, accum_out=st1[:]),
                       [Xb], [hbb, st1b])
                    if SUB == 1:
                        return nc
                    rstd_from_sum(st1, st1b, st2, st2b, D, RMS_EPS)
                    if SUB == 2:
                        return nc
                    op(DVE, lambda: nc.vector.scalar_tensor_tensor(out=hb[:], in0=Xt[:], scalar=st2[:, 0:1], in1=nmB[:],
                                                                   op0=ALU.mult, op1=ALU.mult), [Xb, st2b, nmBb], [hbb])
                    if SUB == 3:
                        return nc
                    transpose16(hb, hbb, hT, hTb, t * 128)
                if STOP == 2:
                    return nc
                blocks = []
                for hd in range(8):
                    if own:
                        blocks.append(("head", hd, [hd * 128, 1024 + hd * 128, 2048 + hd * 128, 3072 + hd * 128]))
                    elif hd % 2 == 0:
                        blocks.append(("head2", hd // 2, [1024 + hd * 128, 2048 + hd * 128, 1024 + (hd + 1) * 128, 2048 + (hd + 1) * 128]))
                if own:
                    for ub in range(2):
                        blocks.append(("u", ub, [4096 + ub * 512 + j * 128 for j in range(4)]))
                    for vbk in range(2):
                        blocks.append(("v", vbk, [5120 + vbk * 512 + j * 128 for j in range(4)]))
                loaded = {}
                loaded[0] = load_block(w_in, blocks[0][2])
                for bi, (kind, idx, cols) in enumerate(blocks):
                    if bi + 1 < len(blocks):
                        loaded[bi + 1] = load_block(w_in, blocks[bi + 1][2])
                    wbt, wbb = loaded.pop(bi)
                    ncols = len(cols) * 128
                    for t in range(GT):
                        zi = zctr[0] % 2
                        zctr[0] += 1
                        zt, zb = inproj(t, wbt, wbb, ncols, zi)
                        if SUB == 11:
                            return nc
                        if kind == "u":
                            op(ACT, lambda: nc.scalar.activation(out=GU[t][0][:, idx * 512:(idx + 1) * 512], in_=zt[:, 0:512], func=AF.Gelu),
                               [zb], [GU[t][1]])
                            continue
                        if kind == "v":
                            op(ACT, lambda: nc.scalar.activation(out=GV[t][0][:, idx * 512:(idx + 1) * 512], in_=zt[:, 0:512], func=AF.Gelu),
                               [zb], [GV[t][1]])
                            continue
                        if kind == "head":
                            hgrn_unit(t, idx, True, zb, zt[:, 0:128], zt[:, 128:256], zt[:, 256:384], zt[:, 384:512])
                        else:
                            for k2 in range(2):
                                hgrn_unit(t, idx * 2 + k2, False, zb, None, zt[:, k2 * 256:k2 * 256 + 128],
                                          zt[:, k2 * 256 + 128:k2 * 256 + 256], None)
                    if STOP == 3:
                        return nc
                if STOP == 4:
                    return nc
                if not own:
                    continue
                if STOP == 5:
                    return nc
                for t in range(GT):
                    GVt, GVb = GV[t]
                    op(DVE, lambda: nc.vector.reduce_sum(out=st1[:], in_=GVt[:], axis=AX.X), [GVb], [st1b])
                    op(ACT, lambda: nc.scalar.activation(out=hb[:, 0:1024], in_=GVt[:], func=AF.Square, accum_out=st3[:]),
                       [GVb], [hbb, st3b])
                    op(DVE, lambda: nc.vector.tensor_scalar(out=st1[:], in0=st1[:], scalar1=1.0 / 1024, scalar2=None, op0=ALU.mult),
                       [st1b], [st1b])
                    op(DVE, lambda: nc.vector.tensor_tensor(out=st5[:], in0=st1[:], in1=st1[:], op=ALU.mult), [st1b], [st5b])
                    op(DVE, lambda: nc.vector.scalar_tensor_tensor(out=st3[:], in0=st3[:], scalar=1.0 / 1024, in1=st5[:],
                                                                   op0=ALU.mult, op1=ALU.subtract), [st3b, st5b], [st3b])
                    rstd_from_sum(st3, st3b, st4, st4b, 1.0, LN_EPS)
                    op(DVE, lambda: nc.vector.tensor_scalar(out=GVt[:], in0=GVt[:], scalar1=st1[:, 0:1], scalar2=st4[:, 0:1],
                                                            op0=ALU.subtract, op1=ALU.mult), [GVb, st1b, st4b], [GVb])
                    op(DVE, lambda: nc.vector.tensor_tensor(out=GVt[:], in0=GVt[:], in1=lngB[:], op=ALU.mult), [GVb, lngBb], [GVb])
                    op(DVE, lambda: nc.vector.tensor_tensor(out=vn[:], in0=GVt[:], in1=lnbB[:], op=ALU.add), [GVb, lnbBb], [vnb])
                    for g8 in range(8):
                        gs = slice(g8 * 128, (g8 + 1) * 128)
                        op(PE, lambda: nc.tensor.matmul(SPp[:, gs], wsT[:, g8, :], vn[:, gs], start=True, stop=True),
                           [wsTb, vnb], [SPa if g8 < 4 else SPbk], mile=(g8 == 3 or g8 == 7))
                    for g8 in range(8):
                        gs = slice(g8 * 128, (g8 + 1) * 128)
                        ms = slice(1024 + g8 * 128, 1024 + (g8 + 1) * 128)
                        op(DVE, lambda: nc.vector.scalar_tensor_tensor(out=mix[t][0][:, ms], in0=SPp[:, gs], scalar=bsT[:, g8:g8 + 1],
                                                                       in1=GU[t][0][:, gs], op0=ALU.add, op1=ALU.mult),
                           [SPa if g8 < 4 else SPbk, bsTb, GU[t][1]], [mix[t][1]])
                if STOP == 6:
                    return nc
                for t in range(GT):
                    transpose16(mix[t][0], mix[t][1], hT, hTb, t * 128)
                oblocks = [[db * 512 + j * 128 for j in range(4)] for db in range(4)]
                loaded = {0: load_block(w_out, oblocks[0], fold_hon=True)}
                for db in range(4):
                    if db + 1 < 4:
                        loaded[db + 1] = load_block(w_out, oblocks[db + 1], fold_hon=True)
                    wbt, wbb = loaded.pop(db)
                    for t in range(GT):
                        zi = zctr[0] % 2
                        zctr[0] += 1
                        zt, zb = inproj(t, wbt, wbb, 512, zi)
                        Xt, Xb = X[t]
                        ds = slice(db * 512, (db + 1) * 512)
                        op(DVE, lambda: nc.vector.tensor_tensor(out=Xt[:, ds], in0=Xt[:, ds], in1=zt[:, 0:512], op=ALU.add), [Xb, zb], [Xb])
                for t in range(GT):
                    To = g * GT + t - NT_PRE
                    dma(x1s[To * 128:(To + 1) * 128, :], X[t][0][:], [X[t][1]], [x1b[To]], X[t][1])

        if STOP == 7:
            return nc
        with ExitStack() as pb:
            def sb(name, shape, dt=F32):
                return pb.enter_context(nc.sbuf_tensor(name, shape, dt)), Buf(name)

            def psb(name, shape, dt=F32):
                return pb.enter_context(nc.psum_tensor(name, shape, dt))

            Y = [sb("Y%d" % i, [128, D]) for i in range(TG)]
            h2T, h2Tb = sb("h2T", [128, 16, TG * 128], BF16)
            aT = [sb("aT%d" % i, [128, 4, max(TG * 128, 1024)], BF16) for i in range(1)]
            gst = [sb("gst%d" % i, [128, 4, 256]) for i in range(4)]
            gub = [sb("gub%d" % i, [128, 16, 256], BF16) for i in range(2)]
            dst_ = [sb("dst%d" % i, [128, 4, 512]) for i in range(2)]
            dnb = [sb("dnb%d" % i, [128, 4, 512], BF16) for i in range(2)]
            tmps = [[sb("at%d_%d" % (i, j), [128, 512]) for j in range(3)] for i in range(2)]
            normB, normBb = sb("normB", [128, D])
            bgu, bgub = sb("bgu", [128, NE * 2 * 16])
            Gt, Gtb = sb("G", [128, TG, NE])
            wrb, wrbb = sb("wrb", [128, 16, NE], BF16)
            bdnb, bdnbb = sb("bdnb", [NE, D], BF16)
            brB, brBb = sb("brB", [128, NE])
            hb, hbb = sb("hb2", [128, D], BF16)
            idb, idbb = sb("idb2", [128, 128], BF16)
            pT, pTb = sb("pT", [128, 2, TG * 128], BF16)
            lg, lgb = sb("lg", [128, NE]); m8, m8b = sb("m8", [128, 8]); msk, mskb = sb("msk", [128, NE])
            ex, exb = sb("ex", [128, NE]); Gb16, Gb16b = sb("Gb16", [128, NE], BF16); GTt, GTtb = sb("GTt", [NE, 128], BF16)
            r1, r1b = sb("r1", [128, 1]); r2, r2b = sb("r2", [128, 1]); r3, r3b = sb("r3", [128, 1]); r4, r4b = sb("r4", [128, 1])

            PG = [(psb("PG%d" % i, [128, 512]), Buf("PG%d" % i, True)) for i in range(2)]
            PU = [(psb("PU%d" % i, [128, 512]), Buf("PU%d" % i, True)) for i in range(2)]
            PY = [(psb("PY%d" % i, [128, 512]), Buf("PY%d" % i, True)) for i in range(2)]
            TR = psb("TR2", [128, 1024])
            TRb = [Buf("TR2a", True), Buf("TR2b", True)]

            op(POOL, lambda: nc.gpsimd.memset(idb[:], 1.0), [], [idbb])
            op(POOL, lambda: nc.gpsimd.affine_select(out=idb[:], in_=idb[:], pattern=[[-1, 128]], compare_op=ALU.is_equal,
                                                     fill=0.0, base=0, channel_multiplier=1), [idbb], [idbb])
            dma(bgu[:], bgu_t[:, :], [], [bgub], bgub)
            bguv = bgu[:].rearrange("p (e two c) -> p e two c", e=NE, two=2)
            op(DVE, lambda: nc.vector.tensor_scalar(out=bguv[:, :, 1, :], in0=bguv[:, :, 1, :], scalar1=1.0, scalar2=None, op0=ALU.add),
               [bgub], [bgub])
            dma(brB[:], b_r.partition_broadcast(128), [], [brBb], brBb)
            y0v = Y[0][0][:, 0:16 * NE].rearrange("p (c n) -> p c n", c=16)
            dma(y0v, w_r.rearrange("(c p) n -> p c n", p=128), [], [Y[0][1]], Y[0][1])
            op(POOL, lambda: nc.gpsimd.tensor_copy(out=wrb[:], in_=y0v), [Y[0][1]], [wrbb])
            dma(Y[1][0][0:NE, :], b_dn[:, :], [], [Y[1][1]], Y[1][1])
            op(POOL, lambda: nc.gpsimd.tensor_copy(out=bdnb[:], in_=Y[1][0][0:NE, :]), [Y[1][1]], [bdnbb])

            def rstd2(src, srcb, dst, dstb, n, eps):
                op(DVE, lambda: nc.vector.tensor_scalar(out=dst[:], in0=src[:], scalar1=1.0 / n, scalar2=eps,
                                                        op0=ALU.mult, op1=ALU.add), [srcb], [dstb])
                op(ACT, lambda: nc.scalar.activation(out=dst[:], in_=dst[:], func=AF.Ln), [dstb], [dstb])
                op(ACT, lambda: nc.scalar.activation(out=dst[:], in_=dst[:], func=AF.Exp, scale=-0.5), [dstb], [dstb])

            def norm_T(t, dstT, dstTb):
                Yt, Yb = Y[t]
                op(ACT, lambda: nc.scalar.activation(out=hb[:], in_=Yt[:], func=AF.Square, accum_out=r1[:]), [Yb], [hbb, r1b])
                rstd2(r1, r1b, r2, r2b, D, RMS_EPS)
                op(DVE, lambda: nc.vector.scalar_tensor_tensor(out=hb[:], in0=Yt[:], scalar=r2[:, 0:1], in1=normB[:],
                                                               op0=ALU.mult, op1=ALU.mult), [Yb, r2b, normBb], [hbb])
                for q4 in range(4):
                    trb = TRb[q4 % 2]
                    base = (q4 % 2) * 512
                    for j in range(4):
                        c = q4 * 4 + j
                        op(PE, lambda c=c, j=j: nc.tensor.matmul(TR[:, base + j * 128: base + (j + 1) * 128],
                                                                    hb[:, c * 128:(c + 1) * 128], idb[:], start=True, stop=True),
                           [hbb, idbb], [trb], mile=(j == 3))
                    srcv = TR[:, base:base + 512].rearrange("p (c t) -> p c t", c=4)
                    dstv = dstT[:, q4 * 4:(q4 + 1) * 4, t * 128:(t + 1) * 128]
                    if q4 % 2 == 0:
                        op(ACT, lambda: nc.scalar.copy(out=dstv, in_=srcv), [trb], [dstTb])
                    else:
                        op(DVE, lambda: nc.vector.tensor_copy(out=dstv, in_=srcv), [trb], [dstTb])

            gctr = {"gst": 0, "gub": 0, "dst": 0, "dnb": 0, "pg": 0, "py": 0, "tmp": 0, "aT": 0}

            def load_gu(src3, deint):
                slot = gctr["gub"] % 2
                gctr["gub"] += 1
                gt_, gb_ = gub[slot]
                for kh in range(4):
                    s = gctr["gst"] % 4
                    gctr["gst"] += 1
                    stt, stb = gst[s]
                    dma(stt[:], src3[kh * 512:(kh + 1) * 512, :].rearrange("(c p) n -> p c n", p=128), [], [stb], stb)
                    if deint:
                        iv = stt[:].rearrange("p c (f two) -> p c two f", two=2)
                        ov = gt_[:, kh * 4:(kh + 1) * 4, :].rearrange("p c (two f) -> p c two f", two=2)
                    else:
                        iv = stt[:]
                        ov = gt_[:, kh * 4:(kh + 1) * 4, :]
                    op(ACT, lambda: nc.scalar.copy(out=ov, in_=iv), [stb], [gb_])
                return gt_, gb_

            def load_dn(e, qt, db):
                slot = gctr["dnb"] % 2
                gctr["dnb"] += 1
                s = gctr["dst"] % 2
                gctr["dst"] += 1
                stt, stb = dst_[s]
                dt_, db_ = dnb[slot]
                dma(stt[:], w_dn[e, qt * 512:(qt + 1) * 512, db * 512:(db + 1) * 512].rearrange("(c p) n -> p c n", p=128),
                    [], [stb], stb)
                op(POOL, lambda: nc.gpsimd.tensor_copy(out=dt_[:], in_=stt[:]), [stb], [db_])
                return dt_, db_

            for grp in range(NT_OWN // TG):
                dma(normB[:], norm_ffn.partition_broadcast(128), [], [normBb], normBb)
                for t in range(TG):
                    To = grp * TG + t
                    Yt, Yb = Y[t]
                    dma(Yt[:], x1s[To * 128:(To + 1) * 128, :], [x1b[To]], [Yb], Yb)
                    norm_T(t, h2T, h2Tb)
                    ts_ = slice(t * 128, (t + 1) * 128)
                    PL, PLb = PY[gctr["py"] % 2]
                    gctr["py"] += 1
                    for c in range(16):
                        op(PE, lambda c=c: nc.tensor.matmul(PL[:, 0:NE], h2T[:, c, ts_], wrb[:, c, :], start=(c == 0), stop=(c == 15)),
                           [h2Tb, wrbb], [PLb], mile=(c == 15))
                    op(DVE, lambda: nc.vector.tensor_tensor(out=lg[:], in0=PL[:, 0:NE], in1=brB[:], op=ALU.add), [PLb, brBb], [lgb])
                    op(DVE, lambda: nc.vector.max(out=m8[:], in_=lg[:]), [lgb], [m8b])
                    op(DVE, lambda: nc.vector.tensor_scalar(out=msk[:], in0=lg[:], scalar1=m8[:, 3:4], scalar2=None, op0=ALU.is_ge),
                       [lgb, m8b], [mskb])
                    op(DVE, lambda: nc.vector.tensor_scalar(out=r3[:], in0=m8[:, 0:1], scalar1=-1.0, scalar2=None, op0=ALU.mult),
                       [m8b], [r3b])
                    op(ACT, lambda: nc.scalar.activation(out=ex[:], in_=lg[:], func=AF.Exp, bias=r3[:, 0:1], scale=1.0), [lgb, r3b], [exb])
                    op(DVE, lambda: nc.vector.tensor_tensor(out=ex[:], in0=ex[:], in1=msk[:], op=ALU.mult), [exb, mskb], [exb])
                    op(DVE, lambda: nc.vector.reduce_sum(out=r4[:], in_=ex[:], axis=AX.X), [exb], [r4b])
                    op(ACT, lambda: nc.scalar.activation(out=r4[:], in_=r4[:], func=AF.Ln), [r4b], [r4b])
                    op(DVE, lambda: nc.vector.tensor_tensor(out=r3[:], in0=r3[:], in1=r4[:], op=ALU.subtract), [r3b, r4b], [r3b])
                    op(ACT, lambda: nc.scalar.activation(out=ex[:], in_=lg[:], func=AF.Exp, bias=r3[:, 0:1], scale=1.0), [lgb, r3b], [exb])
                    op(DVE, lambda: nc.vector.tensor_tensor(out=Gt[:, t, :], in0=ex[:], in1=msk[:], op=ALU.mult), [exb, mskb], [Gtb])
                    op(POOL, lambda: nc.gpsimd.tensor_copy(out=Gb16[:], in_=Gt[:, t, :]), [Gtb], [Gb16b])
                    op(PE, lambda: nc.tensor.matmul(TR[0:NE, 0:128], Gb16[:], idb[:], start=True, stop=True), [Gb16b, idbb], [TRb[0]])
                    op(ACT, lambda: nc.scalar.copy(out=GTt[:], in_=TR[0:NE, 0:128]), [TRb[0]], [GTtb])
                    for db in range(4):
                        ds = slice(db * 512, (db + 1) * 512)
                        pyt, pyb = PY[gctr["py"] % 2]
                        gctr["py"] += 1
                        op(PE, lambda: nc.tensor.matmul(pyt[:], GTt[:], bdnb[:, ds], start=True, stop=True), [GTtb, bdnbb], [pyb])
                        op(DVE, lambda: nc.vector.tensor_tensor(out=Yt[:, ds], in0=Yt[:, ds], in1=pyt[:], op=ALU.add), [Yb, pyb], [Yb])

                if STOP == 8:
                    return nc
                def gu_compute(e, fc, gt_, gb_, at, atb, j):
                    for tg in range(TG // 4):
                        tsl = slice(tg * 512, (tg + 1) * 512)
                        pi = gctr["pg"] % 2
                        gctr["pg"] += 1
                        pgt, pgb = PG[pi]
                        put, pub = PU[pi]
                        for c in range(16):
                            op(PE, lambda c=c: nc.tensor.matmul(pgt[:], gt_[:, c, 0:128], h2T[:, c, tsl], start=(c == 0), stop=(c == 15)),
                               [gb_, h2Tb], [pgb], mile=(c == 15))
                        for c in range(16):
                            op(PE, lambda c=c: nc.tensor.matmul(put[:], gt_[:, c, 128:256], h2T[:, c, tsl], start=(c == 0), stop=(c == 15)),
                               [gb_, h2Tb], [pub], mile=(c == 15))
                        tm = tmps[gctr["tmp"] % 2]
                        gctr["tmp"] += 1
                        (gc, gcb), (sg, sgb), (xx, xxb) = tm
                        ig = (e * 2 + 0) * 16 + fc
                        iu = (e * 2 + 1) * 16 + fc
                        op(DVE, lambda: nc.vector.tensor_scalar(out=gc[:], in0=pgt[:], scalar1=bgu[:, ig:ig + 1], scalar2=7.0,
                                                                op0=ALU.add, op1=ALU.min), [pgb, bgub], [gcb])
                        op(ACT, lambda: nc.scalar.activation(out=sg[:], in_=gc[:], func=AF.Sigmoid, scale=1.702), [gcb], [sgb])
                        op(DVE, lambda: nc.vector.tensor_scalar(out=xx[:], in0=put[:], scalar1=bgu[:, iu:iu + 1], scalar2=-6.0,
                                                                op0=ALU.add, op1=ALU.max), [pub, bgub], [xxb])
                        op(POOL, lambda: nc.gpsimd.tensor_tensor(out=gc[:], in0=gc[:], in1=sg[:], op=ALU.mult), [gcb, sgb], [gcb])
                        op(DVE, lambda: nc.vector.scalar_tensor_tensor(out=at[:, j, tsl], in0=xx[:], scalar=8.0, in1=gc[:],
                                                                       op0=ALU.min, op1=ALU.mult), [xxb, gcb], [atb])

                def dn_compute(e, dt_, db_, at, atb, db):
                    ds = slice(db * 512, (db + 1) * 512)
                    for t in range(TG):
                        ts_ = slice(t * 128, (t + 1) * 128)
                        pyt, pyb = PY[gctr["py"] % 2]
                        gctr["py"] += 1
                        for j in range(4):
                            op(PE, lambda j=j: nc.tensor.matmul(pyt[:], at[:, j, ts_], dt_[:, j, :], start=(j == 0), stop=(j == 3)),
                               [atb, db_], [pyb], mile=(j == 3))
                        Yt, Yb = Y[t]
                        op(DVE, lambda: nc.vector.scalar_tensor_tensor(out=Yt[:, ds], in0=pyt[:], scalar=Gt[:, t, e:e + 1], in1=Yt[:, ds],
                                                                       op0=ALU.mult, op1=ALU.add), [pyb, Gtb, Yb], [Yb])

                steps = []
                for e in range(NE):
                    for qt in range(4):
                        for j in range(4):
                            steps.append(("gu", e, qt, j))
                        for db in range(4):
                            steps.append(("dn", e, qt, db))

                def issue_load(st):
                    kind, e, qt, j = st
                    if kind == "gu":
                        fc = qt * 4 + j
                        return load_gu(w_gu[e, :, fc * 256:(fc + 1) * 256], True)
                    return load_dn(e, qt, j)

                pend = {0: issue_load(steps[0])}
                cur_at = None
                for si, st in enumerate(steps):
                    if si + 1 < len(steps):
                        pend[si + 1] = issue_load(steps[si + 1])
                    wt_, wb_ = pend.pop(si)
                    kind, e, qt, j = st
                    if kind == "gu":
                        if j == 0:
                            cur_at = aT[0]
                        gu_compute(e, qt * 4 + j, wt_, wb_, cur_at[0], cur_at[1], j)
                    else:
                        dn_compute(e, wt_, wb_, cur_at[0], cur_at[1], j)

                if STOP == 9:
                    return nc
                dma(normB[:], norm_ple.partition_broadcast(128), [], [normBb], normBb)
                wpp_t, wpp_b = aT[0]
                wppv = wpp_t[:].rearrange("p a b -> p (a b)")[:, 0:4096].rearrange("p (c n) -> p c n", c=2)
                for c2 in range(2):
                    stt, stb = gst[c2]
                    for hh in range(2):
                        sv = stt[:].rearrange("p a b -> p (a b)")
                        dma(sv, w_pp[c2 * 128:(c2 + 1) * 128, hh * 1024:(hh + 1) * 1024], [], [stb], stb)
                        op(POOL, lambda: nc.gpsimd.tensor_copy(out=wppv[:, c2, hh * 1024:(hh + 1) * 1024], in_=sv), [stb], [wpp_b])
                for t in range(TG):
                    To = grp * TG + t
                    norm_T(t, h2T, h2Tb)
                    pt, ptb = tmps[0][0]
                    dma(pt[:, 0:256], pin[To * 128:(To + 1) * 128, :], [], [ptb], ptb)
                    op(POOL, lambda: nc.gpsimd.tensor_copy(out=hb[:, 0:256], in_=pt[:, 0:256]), [ptb], [hbb])
                    for c2 in range(2):
                        op(PE, lambda: nc.tensor.matmul(TR[:, c2 * 128:(c2 + 1) * 128], hb[:, c2 * 128:(c2 + 1) * 128], idb[:], start=True, stop=True),
                           [hbb, idbb], [TRb[0]], mile=(c2 == 1))
                    op(DVE, lambda: nc.vector.tensor_copy(out=pT[:, :, t * 128:(t + 1) * 128],
                                                          in_=TR[:, 0:256].rearrange("p (c t) -> p c t", c=2)), [TRb[0]], [pTb])
                for gb8 in range(8):
                    gt_, gb_ = load_gu(w_pg[:, gb8 * 256:(gb8 + 1) * 256], False)
                    cs = slice(gb8 * 256, (gb8 + 1) * 256)
                    for t in range(TG):
                        ts_ = slice(t * 128, (t + 1) * 128)
                        pi = gctr["pg"] % 2
                        gctr["pg"] += 1
                        pgt, pgb = PG[pi]
                        put, pub = PU[pi]
                        for c in range(16):
                            op(PE, lambda c=c: nc.tensor.matmul(pgt[:, 0:256], h2T[:, c, ts_], gt_[:, c, :], start=(c == 0), stop=(c == 15)),
                               [h2Tb, gb_], [pgb], mile=(c == 15))
                        for c2 in range(2):
                            op(PE, lambda c2=c2: nc.tensor.matmul(put[:, 0:256], pT[:, c2, ts_], wppv[:, c2, cs], start=(c2 == 0), stop=(c2 == 1)),
                               [pTb, wpp_b], [pub], mile=(c2 == 1))
                        tm = tmps[gctr["tmp"] % 2]
                        gctr["tmp"] += 1
                        (gc, gcb), (sg, sgb) = tm[0], tm[1]
                        Yt, Yb = Y[t]
                        op(ACT, lambda: nc.scalar.activation(out=gc[:, 0:256], in_=pgt[:, 0:256], func=AF.Sigmoid), [pgb], [gcb])
                        op(DVE, lambda: nc.vector.tensor_tensor(out=sg[:, 0:256], in0=gc[:, 0:256], in1=put[:, 0:256], op=ALU.mult),
                           [gcb, pub], [sgb])
                        op(DVE, lambda: nc.vector.tensor_tensor(out=Yt[:, cs], in0=Yt[:, cs], in1=sg[:, 0:256], op=ALU.add), [Yb, sgb], [Yb])
                dma(normB[:], norm_final.partition_broadcast(128), [], [normBb], normBb)
                for t in range(TG):
                    To = grp * TG + t
                    Yt, Yb = Y[t]
                    op(ACT, lambda: nc.scalar.activation(out=hb[:], in_=Yt[:], func=AF.Square, accum_out=r1[:]), [Yb], [hbb, r1b])
                    rstd2(r1, r1b, r2, r2b, D, RMS_EPS)
                    op(DVE, lambda: nc.vector.scalar_tensor_tensor(out=Yt[:], in0=Yt[:], scalar=r2[:, 0:1], in1=normB[:],
                                                                   op0=ALU.mult, op1=ALU.mult), [Yb, r2b, normBb], [Yb])
                    ob = Buf("o%d" % To)
                    out_toks.append(dma(out[To * 128:(To + 1) * 128, :], Yt[:], [Yb], [ob], Yb))
            for tok in out_toks:
                SP.wait(tok)
    return nc


def _layouts(inputs):
    f = lambda a: np.ascontiguousarray(np.asarray(a, dtype=np.float32))
    x = f(inputs["x"]); p = f(inputs["p"])
    shared = {
        "w_in": f(inputs["w_in"][0]), "w_out": f(inputs["w_out"][0]),
        "w_gu": f(inputs["w_gate_up"][0]), "w_dn": f(inputs["w_down"][0]),
        "w_pg": f(inputs["w_ple_gate"][0]), "w_pp": f(inputs["w_ple_proj"][0]),
        "w_r": f(inputs["w_router"][0]),
        "norm_mix": f(inputs["norm_mix"][0]), "norm_ffn": f(inputs["norm_ffn"][0]),
        "norm_ple": f(inputs["norm_ple"][0]), "norm_final": f(inputs["norm_final"]),
        "lb_logits": f(inputs["lb_logits"]),
        "hon_t": f(np.asarray(inputs["hgrn_out_norm"][0]).reshape(8, 128).T),
        "ln_g": f(inputs["gmlp_ln_g"][0]), "ln_b": f(inputs["gmlp_ln_b"][0]),
        "ws_t": f(np.asarray(inputs["w_spatial"][0]).transpose(2, 0, 1)),
        "bs_t": f(np.asarray(inputs["b_spatial"][0]).T),
        "b_r": f(inputs["b_router"][0]),
        "bgu_t": f(np.asarray(inputs["b_gate_up"][0]).reshape(NE, 16, 128, 2).transpose(2, 0, 3, 1).reshape(128, NE * 2 * 16)),
        "b_dn": f(inputs["b_down"][0]),
    }
    maps = []
    for c in range(8):
        b, half = c // 2, c % 2
        own = x[b, half * 2048:(half + 1) * 2048]
        pre = x[b, 0:2048] if half == 1 else np.zeros_like(own)
        m = dict(shared)
        m["xs"] = np.ascontiguousarray(np.concatenate([pre, own], axis=0))
        m["p"] = np.ascontiguousarray(p[0, b, half * 2048:(half + 1) * 2048])
        maps.append(m)
    return maps


def kernel(**inputs):
    maps = _layouts(inputs)
    nc = build_nc()
    res = run_bass_kernel_spmd(nc, maps, core_ids=list(range(8)))
    outp = np.empty((4, 4096, 2048), np.float32)
    for c in range(8):
        b, half = c // 2, c % 2
        outp[b, half * 2048:(half + 1) * 2048] = np.asarray(res.results[c]["out"], dtype=np.float32)
    return outp
```

```python
import numpy as np
from contextlib import ExitStack
import concourse.bass as bass
import concourse.mybir as mybir
from concourse.bass_utils import run_bass_kernel_spmd

F32 = mybir.dt.float32
BF16 = mybir.dt.bfloat16
AF = mybir.ActivationFunctionType
ALU = mybir.AluOpType
AX = mybir.AxisListType

D = 2048
NT_OWN = 16
NT_PRE = 16
GT = 4
TG = 8
NE = 32
RMS_EPS = 1e-6
LN_EPS = 1e-5
DEBUG = False
STOP = 0
SUB = 0


class Eng:
    def __init__(self, e, sem, is_pe=False):
        self.e = e
        self.sem = sem
        self.cnt = 0
        self.seen = {}
        self.is_pe = is_pe

    def wait(self, tok):
        if tok is None:
            return
        sem, val = tok
        if sem is self.sem and self.is_pe:
            return
        key = id(sem)
        if self.seen.get(key, 0) >= val:
            return
        self.e.wait_ge(sem, val)
        self.seen[key] = val

    def done(self, ins):
        ins.then_inc(self.sem, 1)
        self.cnt += 1
        return (self.sem, self.cnt)

    def future(self):
        return (self.sem, self.cnt + 1)


class Buf:
    def __init__(self, name="", excl=False):
        self.name = name
        self.excl = excl
        self.w = None
        self.r = {}
        self.dsem = None
        self.dcnt = 0


def _deps(E, reads, writes):
    for b in reads:
        E.wait(b.w)
        if b.excl:
            for t in list(b.r.values()):
                if t[0] is not E.sem:
                    E.wait(t)
    for b in writes:
        E.wait(b.w)
        for t in list(b.r.values()):
            E.wait(t)


def _upd(tok, reads, writes):
    for b in reads:
        k = id(tok[0])
        if k not in b.r or b.r[k][1] < tok[1]:
            b.r[k] = tok
    for b in writes:
        b.w = tok
        b.r = {}


def op(E, fn, reads=(), writes=(), mile=True):
    _deps(E, reads, writes)
    ins = fn()
    tok = E.done(ins) if mile else E.future()
    _upd(tok, reads, writes)
    return tok


def build_nc():
    nc = bass.Bass("TRN2", target_bir_lowering=False)

    def din(name, shape):
        return nc.dram_tensor(name, shape, F32, kind="ExternalInput").ap()

    xs = din("xs", [(NT_PRE + NT_OWN) * 128, D])
    pin = din("p", [NT_OWN * 128, 256])
    w_in = din("w_in", [D, 6144])
    w_out = din("w_out", [D, D])
    w_gu = din("w_gu", [NE, D, 4096])
    w_dn = din("w_dn", [NE, D, D])
    w_pg = din("w_pg", [D, D])
    w_pp = din("w_pp", [256, D])
    w_r = din("w_r", [D, NE])
    norm_mix = din("norm_mix", [D])
    norm_ffn = din("norm_ffn", [D])
    norm_ple = din("norm_ple", [D])
    norm_final = din("norm_final", [D])
    lb_logits = din("lb_logits", [2, 1024])
    hon_t = din("hon_t", [128, 8])
    ln_g = din("ln_g", [1024])
    ln_b = din("ln_b", [1024])
    ws_t = din("ws_t", [128, 8, 128])
    bs_t = din("bs_t", [128, 8])
    b_r = din("b_r", [NE])
    bgu_t = din("bgu_t", [128, NE * 2 * 16])
    b_dn = din("b_dn", [NE, D])
    out = nc.dram_tensor("out", [NT_OWN * 128, D], F32, kind="ExternalOutput").ap()
    x1s = nc.dram_tensor("x1s", [NT_OWN * 128, D], F32, kind=("ExternalOutput" if DEBUG else "Internal")).ap()

    es = ExitStack()
    with es:
        def sem(name):
            return es.enter_context(nc.semaphore(name))

        PE = Eng(nc.tensor, sem("s_pe"), is_pe=True)
        ACT = Eng(nc.scalar, sem("s_act"))
        DVE = Eng(nc.vector, sem("s_dve"))
        POOL = Eng(nc.gpsimd, sem("s_pool"))
        SP = Eng(nc.sync, sem("s_sp"))

        def dma(out_ap, in_ap, reads, writes, sb):
            if sb.dsem is None:
                sb.dsem = sem("d_" + sb.name)
            _deps(SP, reads, writes)
            SP.e.dma_start(out=out_ap, in_=in_ap).then_inc(sb.dsem, 16)
            sb.dcnt += 16
            tok = (sb.dsem, sb.dcnt)
            _upd(tok, reads, writes)
            return tok

        x1b = [Buf("x1s%d" % i) for i in range(NT_OWN)]
        out_toks = []

        with ExitStack() as pa:
            def sb(name, shape, dt=F32):
                return pa.enter_context(nc.sbuf_tensor(name, shape, dt)), Buf(name)

            def psb(name, shape, dt=F32):
                return pa.enter_context(nc.psum_tensor(name, shape, dt))

            X = [sb("X%d" % i, [128, D]) for i in range(GT)]
            hT, hTb = sb("hT", [128, 16, GT * 128], BF16)
            hb, hbb = sb("hb", [128, D], BF16)
            stg = [sb("stg%d" % i, [128, 16, 128]) for i in range(3)]
            wb = [sb("wb%d" % i, [128, 16, 512], BF16) for i in range(2)]
            mix = [sb("mix%d" % i, [128, D], BF16) for i in range(GT)]
            GU = [sb("GU%d" % i, [128, 1024]) for i in range(GT)]
            GV = [sb("GV%d" % i, [128, 1024]) for i in range(GT)]
            vn, vnb = sb("vn", [128, 1024], BF16)
            nmB, nmBb = sb("nmB", [128, D])
            lbB, lbBb = sb("lbB", [128, 1024])
            omlB, omlBb = sb("omlB", [128, 1024])
            lngB, lngBb = sb("lngB", [128, 1024])
            lnbB, lnbBb = sb("lnbB", [128, 1024])
            wsT, wsTb = sb("wsT", [128, 8, 128], BF16)
            bsT, bsTb = sb("bsT", [128, 8])
            hon, honb = sb("hon", [128, 8])
            idb, idbb = sb("idb", [128, 128], BF16)
            triF, triFb = sb("triF", [128, 128])
            ind, indb = sb("ind", [128, 2])
            S = [sb("S%d" % i, [128, 128]) for i in range(8)]
            Sb = [sb("Sb%d" % i, [128, 128], BF16) for i in range(8)]
            def mkset(i):
                d = {}
                for nm in ["sf", "sq", "sgt", "ff", "lf", "kk", "enb", "eb", "qs", "tmpS"]:
                    d[nm] = sb("%s_%d" % (nm, i), [128, 128])
                d["dec"] = sb("dec_%d" % i, [128, 2])
                for nm in ["vbt", "ke", "qe", "keT", "qeT", "scm"]:
                    d[nm] = sb("%s_%d" % (nm, i), [128, 128], BF16)
                d["u3"] = sb("u3_%d" % i, [128, 1])
                d["u4"] = sb("u4_%d" % i, [128, 1])
                return d
            TS = [mkset(0), mkset(1)]
            uctr = [0]
            st1, st1b = sb("st1", [128, 1]); st2, st2b = sb("st2", [128, 1]); st3, st3b = sb("st3", [128, 1])
            st4, st4b = sb("st4", [128, 1]); st5, st5b = sb("st5", [128, 1])

            Z = [(psb("Z%d" % i, [128, 512]), Buf("Z%d" % i, True)) for i in range(2)]
            UA = [psb("UA%d" % i, [128, 512]) for i in range(2)]
            UAb = [Buf("UA%d" % i, True) for i in range(2)]
            UB = [psb("UB%d" % i, [128, 512]) for i in range(2)]
            UBb = [Buf("UB%d" % i, True) for i in range(2)]
            SPa = Buf("SPa", True)
            SPbk = Buf("SPbk", True)
            SPp = psb("SPp", [128, 1024])

            cst = Buf("cst")

            dma(nmB[:], norm_mix.partition_broadcast(128), [], [nmBb], nmBb)
            dma(lbB[:], lb_logits[0, :].partition_broadcast(128), [], [lbBb], lbBb)
            dma(omlB[:], lb_logits[1, :].partition_broadcast(128), [], [omlBb], omlBb)
            dma(lngB[:], ln_g.partition_broadcast(128), [], [lngBb], lngBb)
            dma(lnbB[:], ln_b.partition_broadcast(128), [], [lnbBb], lnbBb)
            dma(bsT[:], bs_t[:, :], [], [bsTb], bsTb)
            dma(hon[:], hon_t[:, :], [], [honb], honb)
            gu0v = GU[0][0][:].rearrange("p (g t) -> p g t", g=8)
            dma(gu0v, ws_t[:, :, :], [], [GU[0][1]], GU[0][1])
            op(POOL, lambda: nc.gpsimd.affine_select(out=gu0v, in_=gu0v, pattern=[[0, 8], [1, 128]],
                                                     compare_op=ALU.is_ge, fill=0.0, base=0, channel_multiplier=-1),
               [GU[0][1]], [GU[0][1]])
            op(POOL, lambda: nc.gpsimd.tensor_copy(out=wsT[:], in_=gu0v), [GU[0][1]], [wsTb])
            op(POOL, lambda: nc.gpsimd.memset(idb[:], 1.0), [], [idbb])
            op(POOL, lambda: nc.gpsimd.affine_select(out=idb[:], in_=idb[:], pattern=[[-1, 128]], compare_op=ALU.is_equal,
                                                     fill=0.0, base=0, channel_multiplier=1), [idbb], [idbb])
            op(POOL, lambda: nc.gpsimd.memset(triF[:], 1.0), [], [triFb])
            op(POOL, lambda: nc.gpsimd.affine_select(out=triF[:], in_=triF[:], pattern=[[1, 128]], compare_op=ALU.is_ge,
                                                     fill=0.0, base=0, channel_multiplier=-1), [triFb], [triFb])
            op(POOL, lambda: nc.gpsimd.memset(triF[0:64, 64:128], 0.0), [triFb], [triFb])
            op(POOL, lambda: nc.gpsimd.memset(ind[:], 0.0), [], [indb])
            op(POOL, lambda: nc.gpsimd.memset(ind[0:64, 0:1], 1.0), [indb], [indb])
            op(POOL, lambda: nc.gpsimd.memset(ind[64:128, 1:2], 1.0), [indb], [indb])
            for h in range(8):
                op(POOL, lambda h=h: nc.gpsimd.memset(S[h][0][:], 0.0), [], [S[h][1]])
                op(POOL, lambda h=h: nc.gpsimd.memset(Sb[h][0][:], 0.0), [], [Sb[h][1]])
            op(DVE, lambda: nc.vector.tensor_tensor(out=lbB[:], in0=lbB[:], in1=omlB[:], op=ALU.subtract), [lbBb, omlBb], [lbBb])
            op(ACT, lambda: nc.scalar.activation(out=lbB[:], in_=lbB[:], func=AF.Sigmoid), [lbBb], [lbBb])
            op(DVE, lambda: nc.vector.tensor_scalar(out=omlB[:], in0=lbB[:], scalar1=-1.0, scalar2=1.0, op0=ALU.mult, op1=ALU.add),
               [lbBb], [omlBb])

            if STOP == 1:
                return nc

            def rstd_from_sum(src, srcb, dst, dstb, n, eps):
                op(DVE, lambda: nc.vector.tensor_scalar(out=dst[:], in0=src[:], scalar1=1.0 / n, scalar2=eps,
                                                        op0=ALU.mult, op1=ALU.add), [srcb], [dstb])
                op(ACT, lambda: nc.scalar.activation(out=dst[:], in_=dst[:], func=AF.Ln), [dstb], [dstb])
                op(ACT, lambda: nc.scalar.activation(out=dst[:], in_=dst[:], func=AF.Exp, scale=-0.5), [dstb], [dstb])

            def transpose16(src, srcb, dstT, dstTb, tcol):
                for q4 in range(4):
                    TRt, trb = UB[q4 % 2], UBb[q4 % 2]
                    for j in range(4):
                        c = q4 * 4 + j
                        op(PE, lambda c=c, j=j: nc.tensor.matmul(TRt[:, j * 128:(j + 1) * 128],
                                                                 src[:, c * 128:(c + 1) * 128], idb[:], start=True, stop=True),
                           [srcb, idbb], [trb], mile=(j == 3))
                    srcv = TRt[:, 0:512].rearrange("p (c t) -> p c t", c=4)
                    dstv = dstT[:, q4 * 4:(q4 + 1) * 4, tcol:tcol + 128]
                    if q4 % 2 == 0:
                        op(ACT, lambda: nc.scalar.copy(out=dstv, in_=srcv), [trb], [dstTb])
                    else:
                        op(DVE, lambda: nc.vector.tensor_copy(out=dstv, in_=srcv), [trb], [dstTb])

            wstate = {"stg": 0, "wb": 0}

            def load_block(wmat, cols, fold_hon=False):
                slot = wstate["wb"] % 2
                wstate["wb"] += 1
                wbt, wbb = wb[slot]
                for j, c0 in enumerate(cols):
                    s = wstate["stg"] % 3
                    wstate["stg"] += 1
                    stt, stb = stg[s]
                    dma(stt[:], wmat[:, c0:c0 + 128].rearrange("(c p) n -> p c n", p=128), [], [stb], stb)
                    if fold_hon:
                        honv = hon[:, 0:8].rearrange("p (c o) -> p c o", o=1).to_broadcast([128, 8, 128])
                        op(POOL, lambda: nc.gpsimd.tensor_tensor(out=wbt[:, 0:8, j * 128:(j + 1) * 128], in0=stt[:, 0:8, :],
                                                                 in1=honv, op=ALU.mult), [stb, honb], [wbb])
                        op(POOL, lambda: nc.gpsimd.tensor_copy(out=wbt[:, 8:16, j * 128:(j + 1) * 128], in_=stt[:, 8:16, :]),
                           [stb], [wbb])
                    else:
                        op(POOL, lambda: nc.gpsimd.tensor_copy(out=wbt[:, :, j * 128:(j + 1) * 128], in_=stt[:, :, :]),
                           [stb], [wbb])
                return wbt, wbb

            def inproj(t, wbt, wbb, ncols, zi):
                zt, zb = Z[zi]
                for c in range(16):
                    op(PE, lambda c=c: nc.tensor.matmul(zt[:, 0:ncols], hT[:, c, t * 128:(t + 1) * 128], wbt[:, c, 0:ncols],
                                                        start=(c == 0), stop=(c == 15)),
                       [hTb, wbb], [zb], mile=(c == 15))
                return zt, zb

            def hgrn_unit(t, hd, own, zb, zq, zf, zv, zg):
                hs = slice(hd * 128, (hd + 1) * 128)
                tset = TS[uctr[0] % 2]
                uctr[0] += 1
                (sf, sfb), (sq, sqb), (sgt, sgtb), (ff, ffb), (lf, lfb), (kk, kkb) = (tset[k_] for k_ in ["sf", "sq", "sgt", "ff", "lf", "kk"])
                (enb, enbb), (eb, ebb), (qs, qsb), (tmpS, tmpSb), (dec, decb) = (tset[k_] for k_ in ["enb", "eb", "qs", "tmpS", "dec"])
                (vbt, vbtb), (ke, keb), (qe, qeb), (keT, keTb), (qeT, qeTb), (scm, scmb) = (tset[k_] for k_ in ["vbt", "ke", "qe", "keT", "qeT", "scm"])
                (u3, u3b), (u4, u4b) = tset["u3"], tset["u4"]
                u_ = (uctr[0] - 1) % 2
                UAt, UAk, UBt, UBk = UA[u_], UAb[u_], UB[u_], UBb[u_]
                op(ACT, lambda: nc.scalar.activation(out=sf[:], in_=zf, func=AF.Sigmoid), [zb], [sfb])
                if own:
                    op(ACT, lambda: nc.scalar.activation(out=sq[:], in_=zq, func=AF.Sigmoid), [zb], [sqb])
                    op(ACT, lambda: nc.scalar.activation(out=sgt[:], in_=zg, func=AF.Sigmoid), [zb], [sgtb])
                op(ACT, lambda: nc.scalar.copy(out=vbt[:], in_=zv), [zb], [vbtb])
                op(DVE, lambda: nc.vector.tensor_tensor(out=ff[:], in0=sf[:], in1=omlB[:, hs], op=ALU.mult), [sfb, omlBb], [ffb])
                op(DVE, lambda: nc.vector.tensor_tensor(out=ff[:], in0=ff[:], in1=lbB[:, hs], op=ALU.add), [ffb, lbBb], [ffb])
                op(ACT, lambda: nc.scalar.activation(out=lf[:], in_=ff[:], func=AF.Ln), [ffb], [lfb])
                op(POOL, lambda: nc.gpsimd.tensor_scalar(out=kk[:], in0=ff[:], scalar1=-1.0, scalar2=1.0, op0=ALU.mult, op1=ALU.add),
                   [ffb], [kkb])
                if own:
                    op(DVE, lambda: nc.vector.tensor_tensor(out=qs[:], in0=zq, in1=sq[:], op=ALU.mult), [zb, sqb], [qsb])
                op(PE, lambda: nc.tensor.matmul(UAt[:, 0:128], triF[:], lf[:], start=True, stop=True), [triFb, lfb], [UAk])
                op(PE, lambda: nc.tensor.matmul(UAt[:, 384:386], lf[:], ind[:], start=True, stop=True), [lfb, indb], [UAk])
                op(ACT, lambda: nc.scalar.activation(out=enb[:], in_=UAt[:, 0:128], func=AF.Exp, scale=-1.0), [UAk], [enbb])
                if own:
                    op(ACT, lambda: nc.scalar.activation(out=eb[:], in_=UAt[:, 0:128], func=AF.Exp), [UAk], [ebb])
                op(ACT, lambda: nc.scalar.activation(out=dec[:], in_=UAt[:, 384:386], func=AF.Exp), [UAk], [decb])
                op(DVE, lambda: nc.vector.tensor_tensor(out=ke[:], in0=kk[:], in1=enb[:], op=ALU.mult), [kkb, enbb], [keb])
                if own:
                    op(DVE, lambda: nc.vector.tensor_tensor(out=qe[:], in0=qs[:], in1=eb[:], op=ALU.mult), [qsb, ebb], [qeb])
                    op(PE, lambda: nc.tensor.matmul(UBt[:, 0:128], ke[:], idb[:], start=True, stop=True), [keb, idbb], [UBk])
                    op(PE, lambda: nc.tensor.matmul(UBt[:, 128:256], qe[:], idb[:], start=True, stop=True), [qeb, idbb], [UBk])
                    op(ACT, lambda: nc.scalar.copy(out=keT[:], in_=UBt[:, 0:128]), [UBk], [keTb])
                    op(DVE, lambda: nc.vector.tensor_copy(out=qeT[:], in_=UBt[:, 128:256]), [UBk], [qeTb])
                    op(PE, lambda: nc.tensor.matmul(UAt[:, 128:256], keT[:], qeT[:], start=True, stop=True), [keTb, qeTb], [UAk])
                    op(DVE, lambda: nc.vector.tensor_tensor(out=scm[:], in0=UAt[:, 128:256], in1=triF[:], op=ALU.mult),
                       [UAk, triFb], [scmb])
                St, Stb = S[hd]
                Sbt, Sbb = Sb[hd]
                for ch in range(2):
                    rs = slice(ch * 64, (ch + 1) * 64)
                    if own:
                        op(PE, lambda: nc.tensor.matmul(UAt[rs, 256:384], scm[rs, rs], vbt[rs, :], start=True, stop=False),
                           [scmb, vbtb], [UAk], mile=False)
                        op(PE, lambda: nc.tensor.matmul(UAt[rs, 256:384], qeT[:, rs], Sbt[:], start=False, stop=True),
                           [qeTb, Sbb], [UAk])
                    su = UBt[:, 256 + ch * 128:256 + (ch + 1) * 128]
                    op(PE, lambda: nc.tensor.matmul(su, ke[rs, :], vbt[rs, :], start=True, stop=True), [keb, vbtb], [UBk])
                    op(DVE, lambda: nc.vector.tensor_tensor(out=tmpS[:], in0=su, in1=St[:], op=ALU.add), [UBk, Stb], [tmpSb])
                    op(DVE, lambda: nc.vector.tensor_scalar(out=St[:], in0=tmpS[:], scalar1=dec[:, ch:ch + 1], scalar2=None, op0=ALU.mult),
                       [tmpSb, decb], [Stb])
                    op(POOL, lambda: nc.gpsimd.tensor_copy(out=Sbt[:], in_=St[:]), [Stb], [Sbb])
                if not own:
                    return None

                def tail():
                    op(ACT, lambda: nc.scalar.activation(out=qs[:], in_=UAt[:, 256:384], func=AF.Square, accum_out=u3[:]),
                       [UAk], [qsb, u3b])
                    rstd_from_sum(u3, u3b, u4, u4b, 128, RMS_EPS)
                    op(DVE, lambda: nc.vector.scalar_tensor_tensor(out=mix[t][0][:, hs], in0=UAt[:, 256:384], scalar=u4[:, 0:1],
                                                                   in1=sgt[:], op0=ALU.mult, op1=ALU.mult),
                       [UAk, u4b, sgtb], [mix[t][1]])
                return tail
            zctr = [0]
            pend_tail = [None]
            for g in range((NT_PRE + NT_OWN) // GT):
                own = g >= NT_PRE // GT
                for t in range(GT):
                    Xt, Xb = X[t]
                    T = g * GT + t
                    dma(Xt[:], xs[T * 128:(T + 1) * 128, :], [], [Xb], Xb)
                    op(ACT, lambda: nc.scalar.activation(out=hb[:], in_=Xt[:], func=AF.Square, accum_out=st1[:]),
                       [Xb], [hbb, st1b])
                    if SUB == 1:
                        return nc
                    rstd_from_sum(st1, st1b, st2, st2b, D, RMS_EPS)
                    if SUB == 2:
                        return nc
                    op(DVE, lambda: nc.vector.scalar_tensor_tensor(out=hb[:], in0=Xt[:], scalar=st2[:, 0:1], in1=nmB[:],
                                                                   op0=ALU.mult, op1=ALU.mult), [Xb, st2b, nmBb], [hbb])
                    if SUB == 3:
                        return nc
                    transpose16(hb, hbb, hT, hTb, t * 128)
                if STOP == 2:
                    return nc
                blocks = []
                for hd in range(8):
                    if own:
                        blocks.append(("head", hd, [hd * 128, 1024 + hd * 128, 2048 + hd * 128, 3072 + hd * 128]))
                    elif hd % 2 == 0:
                        blocks.append(("head2", hd // 2, [1024 + hd * 128, 2048 + hd * 128, 1024 + (hd + 1) * 128, 2048 + (hd + 1) * 128]))
                if own:
                    for ub in range(2):
                        blocks.append(("u", ub, [4096 + ub * 512 + j * 128 for j in range(4)]))
                    for vbk in range(2):
                        blocks.append(("v", vbk, [5120 + vbk * 512 + j * 128 for j in range(4)]))
                loaded = {}
                loaded[0] = load_block(w_in, blocks[0][2])
                for bi, (kind, idx, cols) in enumerate(blocks):
                    if bi + 1 < len(blocks):
                        loaded[bi + 1] = load_block(w_in, blocks[bi + 1][2])
                    wbt, wbb = loaded.pop(bi)
                    ncols = len(cols) * 128
                    for t in range(GT):
                        zi = zctr[0] % 2
                        zctr[0] += 1
                        zt, zb = inproj(t, wbt, wbb, ncols, zi)
                        if SUB == 11:
                            return nc
                        if kind == "u":
                            op(ACT, lambda: nc.scalar.activation(out=GU[t][0][:, idx * 512:(idx + 1) * 512], in_=zt[:, 0:512], func=AF.Gelu),
                               [zb], [GU[t][1]])
                            continue
                        if kind == "v":
                            op(ACT, lambda: nc.scalar.activation(out=GV[t][0][:, idx * 512:(idx + 1) * 512], in_=zt[:, 0:512], func=AF.Gelu),
                               [zb], [GV[t][1]])
                            continue
                        if kind == "head":
                            tl = hgrn_unit(t, idx, True, zb, zt[:, 0:128], zt[:, 128:256], zt[:, 256:384], zt[:, 384:512])
                            if pend_tail[0] is not None:
                                pend_tail[0]()
                            pend_tail[0] = tl
                        else:
                            for k2 in range(2):
                                hgrn_unit(t, idx * 2 + k2, False, zb, None, zt[:, k2 * 256:k2 * 256 + 128],
                                          zt[:, k2 * 256 + 128:k2 * 256 + 256], None)
                    if kind != "head" and pend_tail[0] is not None:
                        pend_tail[0]()
                        pend_tail[0] = None
                    if STOP == 3:
                        return nc
                if pend_tail[0] is not None:
                    pend_tail[0]()
                    pend_tail[0] = None
                if STOP == 4:
                    return nc
                if not own:
                    continue
                if STOP == 5:
                    return nc
                for t in range(GT):
                    GVt, GVb = GV[t]
                    op(DVE, lambda: nc.vector.reduce_sum(out=st1[:], in_=GVt[:], axis=AX.X), [GVb], [st1b])
                    op(ACT, lambda: nc.scalar.activation(out=hb[:, 0:1024], in_=GVt[:], func=AF.Square, accum_out=st3[:]),
                       [GVb], [hbb, st3b])
                    op(DVE, lambda: nc.vector.tensor_scalar(out=st1[:], in0=st1[:], scalar1=1.0 / 1024, scalar2=None, op0=ALU.mult),
                       [st1b], [st1b])
                    op(DVE, lambda: nc.vector.tensor_tensor(out=st5[:], in0=st1[:], in1=st1[:], op=ALU.mult), [st1b], [st5b])
                    op(DVE, lambda: nc.vector.scalar_tensor_tensor(out=st3[:], in0=st3[:], scalar=1.0 / 1024, in1=st5[:],
                                                                   op0=ALU.mult, op1=ALU.subtract), [st3b, st5b], [st3b])
                    rstd_from_sum(st3, st3b, st4, st4b, 1.0, LN_EPS)
                    op(DVE, lambda: nc.vector.tensor_scalar(out=GVt[:], in0=GVt[:], scalar1=st1[:, 0:1], scalar2=st4[:, 0:1],
                                                            op0=ALU.subtract, op1=ALU.mult), [GVb, st1b, st4b], [GVb])
                    op(DVE, lambda: nc.vector.tensor_tensor(out=GVt[:], in0=GVt[:], in1=lngB[:], op=ALU.mult), [GVb, lngBb], [GVb])
                    op(DVE, lambda: nc.vector.tensor_tensor(out=vn[:], in0=GVt[:], in1=lnbB[:], op=ALU.add), [GVb, lnbBb], [vnb])
                    for g8 in range(8):
                        gs = slice(g8 * 128, (g8 + 1) * 128)
                        op(PE, lambda: nc.tensor.matmul(SPp[:, gs], wsT[:, g8, :], vn[:, gs], start=True, stop=True),
                           [wsTb, vnb], [SPa if g8 < 4 else SPbk], mile=(g8 == 3 or g8 == 7))
                    for g8 in range(8):
                        gs = slice(g8 * 128, (g8 + 1) * 128)
                        ms = slice(1024 + g8 * 128, 1024 + (g8 + 1) * 128)
                        op(DVE, lambda: nc.vector.scalar_tensor_tensor(out=mix[t][0][:, ms], in0=SPp[:, gs], scalar=bsT[:, g8:g8 + 1],
                                                                       in1=GU[t][0][:, gs], op0=ALU.add, op1=ALU.mult),
                           [SPa if g8 < 4 else SPbk, bsTb, GU[t][1]], [mix[t][1]])
                if STOP == 6:
                    return nc
                for t in range(GT):
                    transpose16(mix[t][0], mix[t][1], hT, hTb, t * 128)
                oblocks = [[db * 512 + j * 128 for j in range(4)] for db in range(4)]
                loaded = {0: load_block(w_out, oblocks[0], fold_hon=True)}
                for db in range(4):
                    if db + 1 < 4:
                        loaded[db + 1] = load_block(w_out, oblocks[db + 1], fold_hon=True)
                    wbt, wbb = loaded.pop(db)
                    for t in range(GT):
                        zi = zctr[0] % 2
                        zctr[0] += 1
                        zt, zb = inproj(t, wbt, wbb, 512, zi)
                        Xt, Xb = X[t]
                        ds = slice(db * 512, (db + 1) * 512)
                        op(DVE, lambda: nc.vector.tensor_tensor(out=Xt[:, ds], in0=Xt[:, ds], in1=zt[:, 0:512], op=ALU.add), [Xb, zb], [Xb])
                for t in range(GT):
                    To = g * GT + t - NT_PRE
                    dma(x1s[To * 128:(To + 1) * 128, :], X[t][0][:], [X[t][1]], [x1b[To]], X[t][1])

        if STOP == 7:
            return nc
        with ExitStack() as pb:
            def sb(name, shape, dt=F32):
                return pb.enter_context(nc.sbuf_tensor(name, shape, dt)), Buf(name)

            def psb(name, shape, dt=F32):
                return pb.enter_context(nc.psum_tensor(name, shape, dt))

            Y = [sb("Y%d" % i, [128, D]) for i in range(TG)]
            h2T, h2Tb = sb("h2T", [128, 16, TG * 128], BF16)
            aT = [sb("aT%d" % i, [128, 4, max(TG * 128, 1024)], BF16) for i in range(1)]
            gst = [sb("gst%d" % i, [128, 4, 256]) for i in range(4)]
            gub = [sb("gub%d" % i, [128, 16, 256], BF16) for i in range(2)]
            dst_ = [sb("dst%d" % i, [128, 4, 512]) for i in range(2)]
            dnb = [sb("dnb%d" % i, [128, 4, 512], BF16) for i in range(2)]
            tmps = [[sb("at%d_%d" % (i, j), [128, 512]) for j in range(3)] for i in range(2)]
            normB, normBb = sb("normB", [128, D])
            bgu, bgub = sb("bgu", [128, NE * 2 * 16])
            Gt, Gtb = sb("G", [128, TG, NE])
            wrb, wrbb = sb("wrb", [128, 16, NE], BF16)
            bdnb, bdnbb = sb("bdnb", [NE, D], BF16)
            brB, brBb = sb("brB", [128, NE])
            hb, hbb = sb("hb2", [128, D], BF16)
            idb, idbb = sb("idb2", [128, 128], BF16)
            pT, pTb = sb("pT", [128, 2, TG * 128], BF16)
            lg, lgb = sb("lg", [128, NE]); m8, m8b = sb("m8", [128, 8]); msk, mskb = sb("msk", [128, NE])
            ex, exb = sb("ex", [128, NE]); Gb16, Gb16b = sb("Gb16", [128, NE], BF16); GTt, GTtb = sb("GTt", [NE, 128], BF16)
            r1, r1b = sb("r1", [128, 1]); r2, r2b = sb("r2", [128, 1]); r3, r3b = sb("r3", [128, 1]); r4, r4b = sb("r4", [128, 1])

            PG = [(psb("PG%d" % i, [128, 512]), Buf("PG%d" % i, True)) for i in range(2)]
            PU = [(psb("PU%d" % i, [128, 512]), Buf("PU%d" % i, True)) for i in range(2)]
            PY = [(psb("PY%d" % i, [128, 512]), Buf("PY%d" % i, True)) for i in range(2)]
            TR = psb("TR2", [128, 1024])
            TRb = [Buf("TR2a", True), Buf("TR2b", True)]

            op(POOL, lambda: nc.gpsimd.memset(idb[:], 1.0), [], [idbb])
            op(POOL, lambda: nc.gpsimd.affine_select(out=idb[:], in_=idb[:], pattern=[[-1, 128]], compare_op=ALU.is_equal,
                                                     fill=0.0, base=0, channel_multiplier=1), [idbb], [idbb])
            dma(bgu[:], bgu_t[:, :], [], [bgub], bgub)
            bguv = bgu[:].rearrange("p (e two c) -> p e two c", e=NE, two=2)
            op(DVE, lambda: nc.vector.tensor_scalar(out=bguv[:, :, 1, :], in0=bguv[:, :, 1, :], scalar1=1.0, scalar2=None, op0=ALU.add),
               [bgub], [bgub])
            dma(brB[:], b_r.partition_broadcast(128), [], [brBb], brBb)
            y0v = Y[0][0][:, 0:16 * NE].rearrange("p (c n) -> p c n", c=16)
            dma(y0v, w_r.rearrange("(c p) n -> p c n", p=128), [], [Y[0][1]], Y[0][1])
            op(POOL, lambda: nc.gpsimd.tensor_copy(out=wrb[:], in_=y0v), [Y[0][1]], [wrbb])
            dma(Y[1][0][0:NE, :], b_dn[:, :], [], [Y[1][1]], Y[1][1])
            op(POOL, lambda: nc.gpsimd.tensor_copy(out=bdnb[:], in_=Y[1][0][0:NE, :]), [Y[1][1]], [bdnbb])

            def rstd2(src, srcb, dst, dstb, n, eps):
                op(DVE, lambda: nc.vector.tensor_scalar(out=dst[:], in0=src[:], scalar1=1.0 / n, scalar2=eps,
                                                        op0=ALU.mult, op1=ALU.add), [srcb], [dstb])
                op(ACT, lambda: nc.scalar.activation(out=dst[:], in_=dst[:], func=AF.Ln), [dstb], [dstb])
                op(ACT, lambda: nc.scalar.activation(out=dst[:], in_=dst[:], func=AF.Exp, scale=-0.5), [dstb], [dstb])

            def norm_T(t, dstT, dstTb):
                Yt, Yb = Y[t]
                op(ACT, lambda: nc.scalar.activation(out=hb[:], in_=Yt[:], func=AF.Square, accum_out=r1[:]), [Yb], [hbb, r1b])
                rstd2(r1, r1b, r2, r2b, D, RMS_EPS)
                op(DVE, lambda: nc.vector.scalar_tensor_tensor(out=hb[:], in0=Yt[:], scalar=r2[:, 0:1], in1=normB[:],
                                                               op0=ALU.mult, op1=ALU.mult), [Yb, r2b, normBb], [hbb])
                for q4 in range(4):
                    trb = TRb[q4 % 2]
                    base = (q4 % 2) * 512
                    for j in range(4):
                        c = q4 * 4 + j
                        op(PE, lambda c=c, j=j: nc.tensor.matmul(TR[:, base + j * 128: base + (j + 1) * 128],
                                                                    hb[:, c * 128:(c + 1) * 128], idb[:], start=True, stop=True),
                           [hbb, idbb], [trb], mile=(j == 3))
                    srcv = TR[:, base:base + 512].rearrange("p (c t) -> p c t", c=4)
                    dstv = dstT[:, q4 * 4:(q4 + 1) * 4, t * 128:(t + 1) * 128]
                    if q4 % 2 == 0:
                        op(ACT, lambda: nc.scalar.copy(out=dstv, in_=srcv), [trb], [dstTb])
                    else:
                        op(DVE, lambda: nc.vector.tensor_copy(out=dstv, in_=srcv), [trb], [dstTb])

            gctr = {"gst": 0, "gub": 0, "dst": 0, "dnb": 0, "pg": 0, "py": 0, "tmp": 0, "aT": 0}

            def load_gu(src3, deint):
                slot = gctr["gub"] % 2
                gctr["gub"] += 1
                gt_, gb_ = gub[slot]
                for kh in range(4):
                    s = gctr["gst"] % 4
                    gctr["gst"] += 1
                    stt, stb = gst[s]
                    dma(stt[:], src3[kh * 512:(kh + 1) * 512, :].rearrange("(c p) n -> p c n", p=128), [], [stb], stb)
                    if deint:
                        iv = stt[:].rearrange("p c (f two) -> p c two f", two=2)
                        ov = gt_[:, kh * 4:(kh + 1) * 4, :].rearrange("p c (two f) -> p c two f", two=2)
                    else:
                        iv = stt[:]
                        ov = gt_[:, kh * 4:(kh + 1) * 4, :]
                    op(ACT, lambda: nc.scalar.copy(out=ov, in_=iv), [stb], [gb_])
                return gt_, gb_

            def load_dn(e, qt, db):
                slot = gctr["dnb"] % 2
                gctr["dnb"] += 1
                s = gctr["dst"] % 2
                gctr["dst"] += 1
                stt, stb = dst_[s]
                dt_, db_ = dnb[slot]
                dma(stt[:], w_dn[e, qt * 512:(qt + 1) * 512, db * 512:(db + 1) * 512].rearrange("(c p) n -> p c n", p=128),
                    [], [stb], stb)
                op(POOL, lambda: nc.gpsimd.tensor_copy(out=dt_[:], in_=stt[:]), [stb], [db_])
                return dt_, db_

            for grp in range(NT_OWN // TG):
                dma(normB[:], norm_ffn.partition_broadcast(128), [], [normBb], normBb)
                for t in range(TG):
                    To = grp * TG + t
                    Yt, Yb = Y[t]
                    dma(Yt[:], x1s[To * 128:(To + 1) * 128, :], [x1b[To]], [Yb], Yb)
                    norm_T(t, h2T, h2Tb)
                    ts_ = slice(t * 128, (t + 1) * 128)
                    PL, PLb = PY[gctr["py"] % 2]
                    gctr["py"] += 1
                    for c in range(16):
                        op(PE, lambda c=c: nc.tensor.matmul(PL[:, 0:NE], h2T[:, c, ts_], wrb[:, c, :], start=(c == 0), stop=(c == 15)),
                           [h2Tb, wrbb], [PLb], mile=(c == 15))
                    op(DVE, lambda: nc.vector.tensor_tensor(out=lg[:], in0=PL[:, 0:NE], in1=brB[:], op=ALU.add), [PLb, brBb], [lgb])
                    op(DVE, lambda: nc.vector.max(out=m8[:], in_=lg[:]), [lgb], [m8b])
                    op(DVE, lambda: nc.vector.tensor_scalar(out=msk[:], in0=lg[:], scalar1=m8[:, 3:4], scalar2=None, op0=ALU.is_ge),
                       [lgb, m8b], [mskb])
                    op(DVE, lambda: nc.vector.tensor_scalar(out=r3[:], in0=m8[:, 0:1], scalar1=-1.0, scalar2=None, op0=ALU.mult),
                       [m8b], [r3b])
                    op(ACT, lambda: nc.scalar.activation(out=ex[:], in_=lg[:], func=AF.Exp, bias=r3[:, 0:1], scale=1.0), [lgb, r3b], [exb])
                    op(DVE, lambda: nc.vector.tensor_tensor(out=ex[:], in0=ex[:], in1=msk[:], op=ALU.mult), [exb, mskb], [exb])
                    op(DVE, lambda: nc.vector.reduce_sum(out=r4[:], in_=ex[:], axis=AX.X), [exb], [r4b])
                    op(ACT, lambda: nc.scalar.activation(out=r4[:], in_=r4[:], func=AF.Ln), [r4b], [r4b])
                    op(DVE, lambda: nc.vector.tensor_tensor(out=r3[:], in0=r3[:], in1=r4[:], op=ALU.subtract), [r3b, r4b], [r3b])
                    op(ACT, lambda: nc.scalar.activation(out=ex[:], in_=lg[:], func=AF.Exp, bias=r3[:, 0:1], scale=1.0), [lgb, r3b], [exb])
                    op(DVE, lambda: nc.vector.tensor_tensor(out=Gt[:, t, :], in0=ex[:], in1=msk[:], op=ALU.mult), [exb, mskb], [Gtb])
                    op(POOL, lambda: nc.gpsimd.tensor_copy(out=Gb16[:], in_=Gt[:, t, :]), [Gtb], [Gb16b])
                    op(PE, lambda: nc.tensor.matmul(TR[0:NE, 0:128], Gb16[:], idb[:], start=True, stop=True), [Gb16b, idbb], [TRb[0]])
                    op(ACT, lambda: nc.scalar.copy(out=GTt[:], in_=TR[0:NE, 0:128]), [TRb[0]], [GTtb])
                    for db in range(4):
                        ds = slice(db * 512, (db + 1) * 512)
                        pyt, pyb = PY[gctr["py"] % 2]
                        gctr["py"] += 1
                        op(PE, lambda: nc.tensor.matmul(pyt[:], GTt[:], bdnb[:, ds], start=True, stop=True), [GTtb, bdnbb], [pyb])
                        op(DVE, lambda: nc.vector.tensor_tensor(out=Yt[:, ds], in0=Yt[:, ds], in1=pyt[:], op=ALU.add), [Yb, pyb], [Yb])

                if STOP == 8:
                    return nc
                def gu_compute(e, fc, gt_, gb_, at, atb, j):
                    for tg in range(TG // 4):
                        tsl = slice(tg * 512, (tg + 1) * 512)
                        pi = gctr["pg"] % 2
                        gctr["pg"] += 1
                        pgt, pgb = PG[pi]
                        put, pub = PU[pi]
                        for c in range(16):
                            op(PE, lambda c=c: nc.tensor.matmul(pgt[:], gt_[:, c, 0:128], h2T[:, c, tsl], start=(c == 0), stop=(c == 15)),
                               [gb_, h2Tb], [pgb], mile=(c == 15))
                        for c in range(16):
                            op(PE, lambda c=c: nc.tensor.matmul(put[:], gt_[:, c, 128:256], h2T[:, c, tsl], start=(c == 0), stop=(c == 15)),
                               [gb_, h2Tb], [pub], mile=(c == 15))
                        tm = tmps[gctr["tmp"] % 2]
                        gctr["tmp"] += 1
                        (gc, gcb), (sg, sgb), (xx, xxb) = tm
                        ig = (e * 2 + 0) * 16 + fc
                        iu = (e * 2 + 1) * 16 + fc
                        op(DVE, lambda: nc.vector.tensor_scalar(out=gc[:], in0=pgt[:], scalar1=bgu[:, ig:ig + 1], scalar2=7.0,
                                                                op0=ALU.add, op1=ALU.min), [pgb, bgub], [gcb])
                        op(ACT, lambda: nc.scalar.activation(out=sg[:], in_=gc[:], func=AF.Sigmoid, scale=1.702), [gcb], [sgb])
                        op(DVE, lambda: nc.vector.tensor_scalar(out=xx[:], in0=put[:], scalar1=bgu[:, iu:iu + 1], scalar2=-6.0,
                                                                op0=ALU.add, op1=ALU.max), [pub, bgub], [xxb])
                        op(POOL, lambda: nc.gpsimd.tensor_tensor(out=gc[:], in0=gc[:], in1=sg[:], op=ALU.mult), [gcb, sgb], [gcb])
                        op(DVE, lambda: nc.vector.scalar_tensor_tensor(out=at[:, j, tsl], in0=xx[:], scalar=8.0, in1=gc[:],
                                                                       op0=ALU.min, op1=ALU.mult), [xxb, gcb], [atb])

                def dn_compute(e, dt_, db_, at, atb, db):
                    ds = slice(db * 512, (db + 1) * 512)
                    for t in range(TG):
                        ts_ = slice(t * 128, (t + 1) * 128)
                        pyt, pyb = PY[gctr["py"] % 2]
                        gctr["py"] += 1
                        for j in range(4):
                            op(PE, lambda j=j: nc.tensor.matmul(pyt[:], at[:, j, ts_], dt_[:, j, :], start=(j == 0), stop=(j == 3)),
                               [atb, db_], [pyb], mile=(j == 3))
                        Yt, Yb = Y[t]
                        op(DVE, lambda: nc.vector.scalar_tensor_tensor(out=Yt[:, ds], in0=pyt[:], scalar=Gt[:, t, e:e + 1], in1=Yt[:, ds],
                                                                       op0=ALU.mult, op1=ALU.add), [pyb, Gtb, Yb], [Yb])

                steps = []
                for e in range(NE):
                    for qt in range(4):
                        for j in range(4):
                            steps.append(("gu", e, qt, j))
                        for db in range(4):
                            steps.append(("dn", e, qt, db))

                def issue_load(st):
                    kind, e, qt, j = st
                    if kind == "gu":
                        fc = qt * 4 + j
                        return load_gu(w_gu[e, :, fc * 256:(fc + 1) * 256], True)
                    return load_dn(e, qt, j)

                pend = {0: issue_load(steps[0])}
                cur_at = None
                for si, st in enumerate(steps):
                    if si + 1 < len(steps):
                        pend[si + 1] = issue_load(steps[si + 1])
                    wt_, wb_ = pend.pop(si)
                    kind, e, qt, j = st
                    if kind == "gu":
                        if j == 0:
                            cur_at = aT[0]
                        gu_compute(e, qt * 4 + j, wt_, wb_, cur_at[0], cur_at[1], j)
                    else:
                        dn_compute(e, wt_, wb_, cur_at[0], cur_at[1], j)

                if STOP == 9:
                    return nc
                dma(normB[:], norm_ple.partition_broadcast(128), [], [normBb], normBb)
                wpp_t, wpp_b = aT[0]
                wppv = wpp_t[:].rearrange("p a b -> p (a b)")[:, 0:4096].rearrange("p (c n) -> p c n", c=2)
                for c2 in range(2):
                    stt, stb = gst[c2]
                    for hh in range(2):
                        sv = stt[:].rearrange("p a b -> p (a b)")
                        dma(sv, w_pp[c2 * 128:(c2 + 1) * 128, hh * 1024:(hh + 1) * 1024], [], [stb], stb)
                        op(POOL, lambda: nc.gpsimd.tensor_copy(out=wppv[:, c2, hh * 1024:(hh + 1) * 1024], in_=sv), [stb], [wpp_b])
                for t in range(TG):
                    To = grp * TG + t
                    norm_T(t, h2T, h2Tb)
                    pt, ptb = tmps[0][0]
                    dma(pt[:, 0:256], pin[To * 128:(To + 1) * 128, :], [], [ptb], ptb)
                    op(POOL, lambda: nc.gpsimd.tensor_copy(out=hb[:, 0:256], in_=pt[:, 0:256]), [ptb], [hbb])
                    for c2 in range(2):
                        op(PE, lambda: nc.tensor.matmul(TR[:, c2 * 128:(c2 + 1) * 128], hb[:, c2 * 128:(c2 + 1) * 128], idb[:], start=True, stop=True),
                           [hbb, idbb], [TRb[0]], mile=(c2 == 1))
                    op(DVE, lambda: nc.vector.tensor_copy(out=pT[:, :, t * 128:(t + 1) * 128],
                                                          in_=TR[:, 0:256].rearrange("p (c t) -> p c t", c=2)), [TRb[0]], [pTb])
                for gb8 in range(8):
                    gt_, gb_ = load_gu(w_pg[:, gb8 * 256:(gb8 + 1) * 256], False)
                    cs = slice(gb8 * 256, (gb8 + 1) * 256)
                    for t in range(TG):
                        ts_ = slice(t * 128, (t + 1) * 128)
                        pi = gctr["pg"] % 2
                        gctr["pg"] += 1
                        pgt, pgb = PG[pi]
                        put, pub = PU[pi]
                        for c in range(16):
                            op(PE, lambda c=c: nc.tensor.matmul(pgt[:, 0:256], h2T[:, c, ts_], gt_[:, c, :], start=(c == 0), stop=(c == 15)),
                               [h2Tb, gb_], [pgb], mile=(c == 15))
                        for c2 in range(2):
                            op(PE, lambda c2=c2: nc.tensor.matmul(put[:, 0:256], pT[:, c2, ts_], wppv[:, c2, cs], start=(c2 == 0), stop=(c2 == 1)),
                               [pTb, wpp_b], [pub], mile=(c2 == 1))
                        tm = tmps[gctr["tmp"] % 2]
                        gctr["tmp"] += 1
                        (gc, gcb), (sg, sgb) = tm[0], tm[1]
                        Yt, Yb = Y[t]
                        op(ACT, lambda: nc.scalar.activation(out=gc[:, 0:256], in_=pgt[:, 0:256], func=AF.Sigmoid), [pgb], [gcb])
                        op(DVE, lambda: nc.vector.tensor_tensor(out=sg[:, 0:256], in0=gc[:, 0:256], in1=put[:, 0:256], op=ALU.mult),
                           [gcb, pub], [sgb])
                        op(DVE, lambda: nc.vector.tensor_tensor(out=Yt[:, cs], in0=Yt[:, cs], in1=sg[:, 0:256], op=ALU.add), [Yb, sgb], [Yb])
                dma(normB[:], norm_final.partition_broadcast(128), [], [normBb], normBb)
                for t in range(TG):
                    To = grp * TG + t
                    Yt, Yb = Y[t]
                    op(ACT, lambda: nc.scalar.activation(out=hb[:], in_=Yt[:], func=AF.Square, accum_out=r1[:]), [Yb], [hbb, r1b])
                    rstd2(r1, r1b, r2, r2b, D, RMS_EPS)
                    op(DVE, lambda: nc.vector.scalar_tensor_tensor(out=Yt[:], in0=Yt[:], scalar=r2[:, 0:1], in1=normB[:],
                                                                   op0=ALU.mult, op1=ALU.mult), [Yb, r2b, normBb], [Yb])
                    ob = Buf("o%d" % To)
                    out_toks.append(dma(out[To * 128:(To + 1) * 128, :], Yt[:], [Yb], [ob], Yb))
            for tok in out_toks:
                SP.wait(tok)
    return nc


def _layouts(inputs):
    f = lambda a: np.ascontiguousarray(np.asarray(a, dtype=np.float32))
    x = f(inputs["x"]); p = f(inputs["p"])
    shared = {
        "w_in": f(inputs["w_in"][0]), "w_out": f(inputs["w_out"][0]),
        "w_gu": f(inputs["w_gate_up"][0]), "w_dn": f(inputs["w_down"][0]),
        "w_pg": f(inputs["w_ple_gate"][0]), "w_pp": f(inputs["w_ple_proj"][0]),
        "w_r": f(inputs["w_router"][0]),
        "norm_mix": f(inputs["norm_mix"][0]), "norm_ffn": f(inputs["norm_ffn"][0]),
        "norm_ple": f(inputs["norm_ple"][0]), "norm_final": f(inputs["norm_final"]),
        "lb_logits": f(inputs["lb_logits"]),
        "hon_t": f(np.asarray(inputs["hgrn_out_norm"][0]).reshape(8, 128).T),
        "ln_g": f(inputs["gmlp_ln_g"][0]), "ln_b": f(inputs["gmlp_ln_b"][0]),
        "ws_t": f(np.asarray(inputs["w_spatial"][0]).transpose(2, 0, 1)),
        "bs_t": f(np.asarray(inputs["b_spatial"][0]).T),
        "b_r": f(inputs["b_router"][0]),
        "bgu_t": f(np.asarray(inputs["b_gate_up"][0]).reshape(NE, 16, 128, 2).transpose(2, 0, 3, 1).reshape(128, NE * 2 * 16)),
        "b_dn": f(inputs["b_down"][0]),
    }
    maps = []
    for c in range(8):
        b, half = c // 2, c % 2
        own = x[b, half * 2048:(half + 1) * 2048]
        pre = x[b, 0:2048] if half == 1 else np.zeros_like(own)
        m = dict(shared)
        m["xs"] = np.ascontiguousarray(np.concatenate([pre, own], axis=0))
        m["p"] = np.ascontiguousarray(p[0, b, half * 2048:(half + 1) * 2048])
        maps.append(m)
    return maps


def kernel(**inputs):
    maps = _layouts(inputs)
    nc = build_nc()
    res = run_bass_kernel_spmd(nc, maps, core_ids=list(range(8)))
    outp = np.empty((4, 4096, 2048), np.float32)
    for c in range(8):
        b, half = c // 2, c % 2
        outp[b, half * 2048:(half + 1) * 2048] = np.asarray(res.results[c]["out"], dtype=np.float32)
    return outp
```
